# Optimizing a Trainium2 kernel written in Bass

```python
import math
import jax, jax.numpy as jnp
from jax import lax
import numpy as np

D_MODEL = 1024
BATCH = 16
SEQ = 2048
DEPTH = 2

N_EVEN = (DEPTH + 1) // 2
N_ODD = DEPTH // 2

MIX_WIDTH = 1024
A_GROUPS = 4
A_GROUP_DIM = 128
A_WIDTH = A_GROUPS * A_GROUP_DIM
CHUNK = 128
SGU_LN_EPS = 1e-5
B_HEADS = 8
B_HEAD_DIM = 64
B_WIDTH = B_HEADS * B_HEAD_DIM
DECAY_LORA = 64
ICLR_LORA = 64
GATE_LORA = 128
B_IN = 3 * B_WIDTH + DECAY_LORA + ICLR_LORA + GATE_LORA
RWKV_GN_EPS = 64e-5
IN0 = 2 * A_WIDTH + B_IN
C_HEADS = 8
C_KV_HEADS = 2
C_GROUP = C_HEADS // C_KV_HEADS
C_HEAD_DIM = 64
C_WIDTH = C_HEADS * C_HEAD_DIM
KV_WIDTH = C_KV_HEADS * C_HEAD_DIM
N_BRANCH = 3
CMP_LEN = 32
CMP_STRIDE = 16
CMP_HIDDEN = 256
SLC_BLK = 64
SEL_TOPK = 8
WIN = 512
QBLK = 128
ROT_DIM = C_HEAD_DIM // 4
ROPE_THETA = 500000.0
D_WIDTH = 512
CONV_W = 3
IN1 = C_WIDTH + 6 * KV_WIDTH + C_HEADS * N_BRANCH + 3 * D_WIDTH
FFN_DIM = 2816
N_EXPERTS = 8
TOP_K = 2
EXPERT_DIM = 1408
MOE_BLK = 256
NORM_EPS = 1e-6
NEG_INF = -1e30
FORCE_BONUS = 1e6

kernel_name = "hybrid_gmlp_rwkv7_nsa_shortconv_moe"


def _split(p, sizes):
    offs = [int(o) for o in np.cumsum(sizes)[:-1]]
    return jnp.split(p, offs, axis=-1)


def _rms_norm(x, g):
    xf = x.astype(jnp.float32)
    y = xf * lax.rsqrt(jnp.mean(xf * xf, axis=-1, keepdims=True) + NORM_EPS)
    return (y * g.astype(jnp.float32)).astype(x.dtype)


def _masked_softmax(s, mask):
    s = jnp.where(mask, s, NEG_INF)
    m = jnp.max(s, axis=-1, keepdims=True)
    p = jnp.where(mask, jnp.exp(s - m), 0.0)
    return p / jnp.maximum(jnp.sum(p, axis=-1, keepdims=True), 1e-30)


def _rope_partial(x, cos, sin):
    half = ROT_DIM // 2
    c = cos[None, :, None, :].astype(x.dtype)
    s = sin[None, :, None, :].astype(x.dtype)
    x1 = x[..., :half]
    x2 = x[..., half:ROT_DIM]
    return jnp.concatenate([x1 * c - x2 * s, x1 * s + x2 * c, x[..., ROT_DIM:]], axis=-1)


def _chunked_sgu(u, v, ln_g, ln_b, w_s, b_s):
    bsz, t, _ = v.shape
    vf = v.reshape(bsz, t, A_GROUPS, A_GROUP_DIM).astype(jnp.float32)
    mu = jnp.mean(vf, axis=-1, keepdims=True)
    var = jnp.mean(jnp.square(vf - mu), axis=-1, keepdims=True)
    vn = ((vf - mu) * lax.rsqrt(var + SGU_LN_EPS)).reshape(bsz, t, A_WIDTH)
    vn = (vn * ln_g + ln_b).astype(v.dtype)
    vc = vn.reshape(bsz, t // CHUNK, CHUNK, A_GROUPS, A_GROUP_DIM)
    causal = jnp.tril(jnp.ones((CHUNK, CHUNK), dtype=bool))
    wm = jnp.where(causal[None], w_s, 0.0).astype(v.dtype)
    mixed = jnp.einsum('gts,bcsgd->bctgd', wm, vc) + b_s.T[:, :, None]
    return u * mixed.reshape(bsz, t, A_WIDTH)


def _rwkv7_scan(r, w, k, v, kk, a):
    bsz, _, h, n = r.shape

    def step(S, inp):
        r_t, w_t, k_t, v_t, kk_t, a_t = inp
        sa = jnp.einsum('bhij,bhj->bhi', S, kk_t)
        S = (S * w_t[:, :, None, :]
             - sa[..., None] * (kk_t * a_t)[:, :, None, :]
             + v_t[..., None] * k_t[:, :, None, :])
        return S, jnp.einsum('bhij,bhj->bhi', S, r_t)

    xs = tuple(jnp.moveaxis(z, 1, 0) for z in (r, w, k, v, kk, a))
    S0 = jnp.zeros((bsz, h, n, n), jnp.float32)
    _, ys = lax.scan(step, S0, xs)
    return jnp.moveaxis(ys, 0, 1)


def _rwkv7_time_mix(pb, mu, w0, w2, a0, a2, g2, k_k, k_a, r_k, gn_g, gn_b):
    bsz, t, _ = pb.shape
    prev = jnp.pad(pb, ((0, 0), (1, 0), (0, 0)))[:, :-1]
    pb = pb + (prev - pb) * mu
    r, k, v, wl, al, gl = _split(pb, [B_WIDTH, B_WIDTH, B_WIDTH, DECAY_LORA, ICLR_LORA, GATE_LORA])
    w = -jax.nn.softplus(-(w0 + jnp.tanh(wl) @ w2)) - 0.5
    decay = jnp.exp(-jnp.exp(w.astype(jnp.float32)))
    a = jax.nn.sigmoid(a0 + al @ a2)
    g = jax.nn.sigmoid(gl) @ g2

    def heads(z):
        return z.reshape(bsz, t, B_HEADS, B_HEAD_DIM).astype(jnp.float32)

    kk = heads(k * k_k)
    kk = kk / jnp.maximum(jnp.sqrt(jnp.sum(kk * kk, axis=-1, keepdims=True)), 1e-12)
    k = k * (1.0 + (a - 1.0) * k_a)
    rh, kh, vh, ah, wh = heads(r), heads(k), heads(v), heads(a), heads(decay)
    y = _rwkv7_scan(rh, wh, kh, vh, kk, ah)
    ym = jnp.mean(y, axis=-1, keepdims=True)
    yv = jnp.mean(jnp.square(y - ym), axis=-1, keepdims=True)
    y = ((y - ym) * lax.rsqrt(yv + RWKV_GN_EPS)).reshape(bsz, t, B_WIDTH)
    y = y * gn_g.astype(jnp.float32) + gn_b.astype(jnp.float32)
    bonus = jnp.sum(rh * kh * r_k.astype(jnp.float32), axis=-1, keepdims=True) * vh
    y = (y + bonus.reshape(bsz, t, B_WIDTH)) * g.astype(jnp.float32)
    return y.astype(pb.dtype)


def _nsa(q, kc, vc, ks, vs, kw, vw, gl, pos_k, pos_v, ck1, ck2, cv1, cv2, cos, sin):
    bsz, t, _ = q.shape
    f32 = jnp.float32
    scale = C_HEAD_DIM ** -0.5
    q = q.reshape(bsz, t, C_HEADS, C_HEAD_DIM)
    kc, vc, ks, vs, kw, vw = (z.reshape(bsz, t, C_KV_HEADS, C_HEAD_DIM) for z in (kc, vc, ks, vs, kw, vw))

    n_cmp = (t - CMP_LEN) // CMP_STRIDE + 1
    idx = np.arange(n_cmp)[:, None] * CMP_STRIDE + np.arange(CMP_LEN)[None, :]

    def compress(z, pos, w1, w2):
        zb = z[:, idx] + pos[None, None, :, None, :]
        zb = zb.transpose(0, 3, 1, 2, 4).reshape(bsz, C_KV_HEADS, n_cmp, CMP_LEN * C_HEAD_DIM)
        return jax.nn.gelu(zb @ w1) @ w2

    k_cmp = compress(kc, pos_k, ck1, ck2)
    v_cmp = compress(vc, pos_v, cv1, cv2).astype(f32)
    cmp_end = jnp.arange(n_cmp) * CMP_STRIDE + CMP_LEN - 1
    n_slc = t // SLC_BLK
    ci = jnp.arange(n_cmp)[:, None] * CMP_STRIDE
    sj = jnp.arange(n_slc)[None, :] * SLC_BLK
    overlap = ((ci < sj + SLC_BLK) & (ci + CMP_LEN > sj)).astype(f32)
    n_sel = min(SEL_TOPK, n_slc)

    q_rot = _rope_partial(q, cos, sin)
    ks = _rope_partial(ks, cos, sin)
    kw = _rope_partial(kw, cos, sin)
    n_qb = t // QBLK

    def to_qblocks(z):
        return z.reshape(bsz, n_qb, QBLK, C_KV_HEADS, C_GROUP, z.shape[-1]).transpose(1, 0, 3, 4, 2, 5)

    qr_b = to_qblocks(q_rot)
    qn_b = to_qblocks(q)
    g_b = to_qblocks(jax.nn.sigmoid(gl.reshape(bsz, t, C_HEADS, N_BRANCH)))
    ks_blk = ks.reshape(bsz, n_slc, SLC_BLK, C_KV_HEADS, C_HEAD_DIM).transpose(0, 3, 1, 2, 4)
    vs_blk = vs.reshape(bsz, n_slc, SLC_BLK, C_KV_HEADS, C_HEAD_DIM).transpose(0, 3, 1, 2, 4)
    pad = ((0, 0), (0, 0), (WIN, 0), (0, 0))
    kw_pad = jnp.pad(kw.transpose(0, 2, 1, 3), pad)
    vw_pad = jnp.pad(vw.transpose(0, 2, 1, 3), pad)
    b_ix = jnp.arange(bsz)[:, None, None, None]
    h_ix = jnp.arange(C_KV_HEADS)[None, :, None, None]
    jb = jnp.arange(n_slc)

    def body(args):
        qr, qn, gt, blk = args
        s0 = blk * QBLK
        t_pos = s0 + jnp.arange(QBLK)
        sc = jnp.einsum('bhgqd,bhnd->bhgqn', qn, k_cmp).astype(f32) * scale
        pc = _masked_softmax(sc, cmp_end[None, :] <= t_pos[:, None])
        o_c = jnp.einsum('bhgqn,bhnd->bhgqd', pc, v_cmp)
        imp = jnp.einsum('bhgqn,nj->bhqj', pc, overlap)
        cur = t_pos // SLC_BLK
        forced = (jb[None] == 0) | (jb[None] == cur[:, None]) | (jb[None] == cur[:, None] - 1)
        imp = jnp.where(jb[None] * SLC_BLK <= t_pos[:, None],
                        imp + jnp.where(forced, FORCE_BONUS, 0.0), NEG_INF)
        _, sel = lax.top_k(imp, n_sel)
        kg = ks_blk[b_ix, h_ix, sel]
        vg = vs_blk[b_ix, h_ix, sel]
        tok = sel[..., None] * SLC_BLK + jnp.arange(SLC_BLK)
        ms = (tok <= t_pos[:, None, None]).reshape(bsz, C_KV_HEADS, 1, QBLK, n_sel * SLC_BLK)
        ss = jnp.einsum('bhgqd,bhqnld->bhgqnl', qr, kg).astype(f32)
        ps = _masked_softmax(ss.reshape(bsz, C_KV_HEADS, C_GROUP, QBLK, n_sel * SLC_BLK) * scale, ms)
        o_s = jnp.einsum('bhgqm,bhqmd->bhgqd', ps,
                         vg.reshape(bsz, C_KV_HEADS, QBLK, n_sel * SLC_BLK, C_HEAD_DIM).astype(f32))
        kwb = lax.dynamic_slice_in_dim(kw_pad, s0, QBLK + WIN, axis=2)
        vwb = lax.dynamic_slice_in_dim(vw_pad, s0, QBLK + WIN, axis=2)
        key_pos = s0 - WIN + jnp.arange(QBLK + WIN)
        diff = t_pos[:, None] - key_pos[None, :]
        mw = (diff >= 0) & (diff < WIN) & (key_pos[None, :] >= 0)
        sw = jnp.einsum('bhgqd,bhkd->bhgqk', qr, kwb).astype(f32) * scale
        pw = _masked_softmax(sw, mw)
        o_w = jnp.einsum('bhgqk,bhkd->bhgqd', pw, vwb.astype(f32))
        gt = gt.astype(f32)
        return (gt[..., 0:1] * o_c + gt[..., 1:2] * o_s + gt[..., 2:3] * o_w).astype(qr.dtype)

    out = lax.map(body, (qr_b, qn_b, g_b, jnp.arange(n_qb)))
    return out.transpose(1, 0, 4, 2, 3, 5).reshape(bsz, t, C_WIDTH)


def _short_conv(bg, cg, hd, conv_w):
    z = cg * hd
    y = lax.conv_general_dilated(z, conv_w[:, None, :].astype(z.dtype), window_strides=(1,),
                                 padding=[(CONV_W - 1, 0)],
                                 dimension_numbers=('NWC', 'WIO', 'NWC'),
                                 feature_group_count=D_WIDTH)
    return bg * y


def _swiglu(h, w_gate, w_up, w_down):
    return (jax.nn.silu(h @ w_gate) * (h @ w_up)) @ w_down


def _moe_swiglu(h, w_router, b_router, w_gate, w_up, w_down):
    bsz, t, d = h.shape
    n = bsz * t
    nk = n * TOP_K
    xf = h.reshape(n, d)
    logits = (xf @ w_router).astype(jnp.float32) + b_router.astype(jnp.float32)
    top_val, top_idx = lax.top_k(logits, TOP_K)
    gates = jax.nn.softmax(top_val, axis=-1)
    e_flat = top_idx.reshape(-1)
    tok_flat = jnp.repeat(jnp.arange(n, dtype=jnp.int32), TOP_K)
    g_flat = gates.reshape(-1)
    order = jnp.argsort(e_flat)
    e_sorted = e_flat[order]
    counts = jax.ops.segment_sum(jnp.ones_like(e_flat), e_flat, num_segments=N_EXPERTS)
    padded = ((counts + MOE_BLK - 1) // MOE_BLK) * MOE_BLK
    p_end = jnp.cumsum(padded)
    p_start = p_end - padded
    u_start = jnp.cumsum(counts) - counts
    dest = p_start[e_sorted] + jnp.arange(nk) - u_start[e_sorted]
    n_blk = (nk + MOE_BLK - 1) // MOE_BLK + N_EXPERTS
    p_rows = n_blk * MOE_BLK
    row_tok = jnp.zeros((p_rows,), jnp.int32).at[dest].set(tok_flat[order])
    row_w = jnp.zeros((p_rows,), jnp.float32).at[dest].set(g_flat[order])
    blk_e = jnp.minimum(jnp.searchsorted(p_end, jnp.arange(n_blk) * MOE_BLK, side='right'), N_EXPERTS - 1)
    xs = xf[row_tok].reshape(n_blk, MOE_BLK, d)

    def expert_block(args):
        xb, e = args
        return (jax.nn.silu(xb @ w_gate[e]) * (xb @ w_up[e])) @ w_down[e]

    yb = lax.map(expert_block, (xs, blk_e)).reshape(p_rows, d)
    out = jnp.zeros((n, d), h.dtype).at[row_tok].add(yb * row_w[:, None].astype(yb.dtype))
    return out.reshape(bsz, t, d)


def setup_inputs(seed: int = 0) -> dict:
    key = jax.random.key(seed)
    keys = iter(jax.random.split(key, 48))

    def nrm(shape, scale):
        return scale * jax.random.normal(next(keys), shape, jnp.float32)

    def gain(shape):
        return 1.0 + nrm(shape, 0.1)

    def unif(shape, lo, hi):
        return jax.random.uniform(next(keys), shape, jnp.float32, lo, hi)

    E, O, D = N_EVEN, N_ODD, D_MODEL
    return {
        "x": nrm((BATCH, SEQ, D), 1.0),
        "e_norm_mix": gain((E, D)),
        "e_w_in": nrm((E, D, IN0), D ** -0.5),
        "sgu_ln_g": gain((E, A_WIDTH)),
        "sgu_ln_b": nrm((E, A_WIDTH), 0.02),
        "sgu_w": nrm((E, A_GROUPS, CHUNK, CHUNK), CHUNK ** -0.5),
        "sgu_b": gain((E, A_GROUPS, CHUNK)),
        "rwkv_mu": unif((E, B_IN), 0.0, 1.0),
        "rwkv_w0": unif((E, B_WIDTH), -2.5, 0.5),
        "rwkv_w2": nrm((E, DECAY_LORA, B_WIDTH), 0.1 * DECAY_LORA ** -0.5),
        "rwkv_a0": nrm((E, B_WIDTH), 0.5),
        "rwkv_a2": nrm((E, ICLR_LORA, B_WIDTH), 0.5 * ICLR_LORA ** -0.5),
        "rwkv_g2": nrm((E, GATE_LORA, B_WIDTH), GATE_LORA ** -0.5),
        "rwkv_k_k": 0.85 + nrm((E, B_WIDTH), 0.1),
        "rwkv_k_a": gain((E, B_WIDTH)),
        "rwkv_r_k": nrm((E, B_HEADS, B_HEAD_DIM), 0.1),
        "rwkv_gn_g": gain((E, B_WIDTH)),
        "rwkv_gn_b": nrm((E, B_WIDTH), 0.02),
        "e_w_out": nrm((E, MIX_WIDTH, D), MIX_WIDTH ** -0.5),
        "e_norm_ffn": gain((E, D)),
        "ffn_w_gate": nrm((E, D, FFN_DIM), D ** -0.5),
        "ffn_w_up": nrm((E, D, FFN_DIM), D ** -0.5),
        "ffn_w_down": nrm((E, FFN_DIM, D), FFN_DIM ** -0.5),
        "o_norm_mix": gain((O, D)),
        "o_w_in": nrm((O, D, IN1), D ** -0.5),
        "nsa_cmp_pos_k": nrm((O, CMP_LEN, C_HEAD_DIM), 0.1),
        "nsa_cmp_pos_v": nrm((O, CMP_LEN, C_HEAD_DIM), 0.1),
        "nsa_cmp_k_w1": nrm((O, CMP_LEN * C_HEAD_DIM, CMP_HIDDEN), (CMP_LEN * C_HEAD_DIM) ** -0.5),
        "nsa_cmp_k_w2": nrm((O, CMP_HIDDEN, C_HEAD_DIM), CMP_HIDDEN ** -0.5),
        "nsa_cmp_v_w1": nrm((O, CMP_LEN * C_HEAD_DIM, CMP_HIDDEN), (CMP_LEN * C_HEAD_DIM) ** -0.5),
        "nsa_cmp_v_w2": nrm((O, CMP_HIDDEN, C_HEAD_DIM), CMP_HIDDEN ** -0.5),
        "conv_w": nrm((O, CONV_W, D_WIDTH), CONV_W ** -0.5),
        "o_w_out": nrm((O, MIX_WIDTH, D), MIX_WIDTH ** -0.5),
        "o_norm_ffn": gain((O, D)),
        "moe_router": nrm((O, D, N_EXPERTS), D ** -0.5),
        "moe_router_b": nrm((O, N_EXPERTS), 0.01),
        "moe_w_gate": nrm((O, N_EXPERTS, D, EXPERT_DIM), D ** -0.5),
        "moe_w_up": nrm((O, N_EXPERTS, D, EXPERT_DIM), D ** -0.5),
        "moe_w_down": nrm((O, N_EXPERTS, EXPERT_DIM, D), EXPERT_DIM ** -0.5),
        "final_norm": gain((D,)),
    }


def reference(x, e_norm_mix, e_w_in, sgu_ln_g, sgu_ln_b, sgu_w, sgu_b, rwkv_mu, rwkv_w0, rwkv_w2,
              rwkv_a0, rwkv_a2, rwkv_g2, rwkv_k_k, rwkv_k_a, rwkv_r_k, rwkv_gn_g, rwkv_gn_b, e_w_out,
              e_norm_ffn, ffn_w_gate, ffn_w_up, ffn_w_down, o_norm_mix, o_w_in, nsa_cmp_pos_k,
              nsa_cmp_pos_v, nsa_cmp_k_w1, nsa_cmp_k_w2, nsa_cmp_v_w1, nsa_cmp_v_w2, conv_w, o_w_out,
              o_norm_ffn, moe_router, moe_router_b, moe_w_gate, moe_w_up, moe_w_down, final_norm):
    t = x.shape[1]
    pos = jnp.arange(t, dtype=jnp.float32)
    inv_freq = ROPE_THETA ** (-jnp.arange(0, ROT_DIM, 2, dtype=jnp.float32) / ROT_DIM)
    ang = pos[:, None] * inv_freq[None, :]
    cos, sin = jnp.cos(ang), jnp.sin(ang)

    h = x
    for layer in range(DEPTH):
        i = layer // 2
        if layer % 2 == 0:
            hn = _rms_norm(h, e_norm_mix[i])
            p = hn @ e_w_in[i]
            pu, pv, pb = _split(p, [A_WIDTH, A_WIDTH, B_IN])
            ya = _chunked_sgu(jax.nn.gelu(pu), jax.nn.gelu(pv), sgu_ln_g[i], sgu_ln_b[i], sgu_w[i], sgu_b[i])
            yb = _rwkv7_time_mix(pb, rwkv_mu[i], rwkv_w0[i], rwkv_w2[i], rwkv_a0[i], rwkv_a2[i],
                                 rwkv_g2[i], rwkv_k_k[i], rwkv_k_a[i], rwkv_r_k[i],
                                 rwkv_gn_g[i], rwkv_gn_b[i])
            h = h + jnp.concatenate([ya.astype(h.dtype), yb.astype(h.dtype)], axis=-1) @ e_w_out[i]
            hn = _rms_norm(h, e_norm_ffn[i])
            h = h + _swiglu(hn, ffn_w_gate[i], ffn_w_up[i], ffn_w_down[i])
        else:
            hn = _rms_norm(h, o_norm_mix[i])
            p = hn @ o_w_in[i]
            q, kc, vc, ks, vs, kw, vw, gl, bg, cg, hd = _split(
                p, [C_WIDTH] + [KV_WIDTH] * 6 + [C_HEADS * N_BRANCH] + [D_WIDTH] * 3)
            yc = _nsa(q, kc, vc, ks, vs, kw, vw, gl, nsa_cmp_pos_k[i], nsa_cmp_pos_v[i],
                      nsa_cmp_k_w1[i], nsa_cmp_k_w2[i], nsa_cmp_v_w1[i], nsa_cmp_v_w2[i], cos, sin)
            yd = _short_conv(bg, cg, hd, conv_w[i])
            h = h + jnp.concatenate([yc.astype(h.dtype), yd.astype(h.dtype)], axis=-1) @ o_w_out[i]
            hn = _rms_norm(h, o_norm_ffn[i])
            h = h + _moe_swiglu(hn, moe_router[i], moe_router_b[i], moe_w_gate[i], moe_w_up[i], moe_w_down[i])
    return _rms_norm(h, final_norm)
```

```python
import contextlib
import math
import numpy as np
import concourse.bass as bass
import concourse.mybir as mybir
from concourse.bass_utils import run_bass_kernel_spmd

F32 = mybir.dt.float32
F32R = mybir.dt.float32r
BF16 = mybir.dt.bfloat16
AF = mybir.ActivationFunctionType
ALU = mybir.AluOpType
AX = mybir.AxisListType

N_DMA_SEMS = 32
NCORES = 8
T = 2048
NTOK = 4096
D = 1024
TP = T + 64


class _Op:
    __slots__ = ("id", "eng", "fn", "deps", "dma", "sig", "sem", "val", "prev_val")


class Prog:
    ENGS = ("pe", "act", "dve", "pool", "sp")

    def __init__(self, nc, st):
        self.nc = nc
        self.ops = []
        self.last_w = {}
        self.readers = {}
        self.esem = {e: st.enter_context(nc.semaphore("s_" + e)) for e in self.ENGS}
        self.dsem = [st.enter_context(nc.semaphore("d_%d" % i)) for i in range(N_DMA_SEMS)]
        with nc.Block() as block:
            @block.sync
            def _(eng):
                for sm in list(self.esem.values()) + self.dsem:
                    eng.sem_clear(sm)
        self.cnt = {e: 0 for e in self.ENGS}
        self.dma_use = [0] * N_DMA_SEMS
        self.ndma = 0
        self.ndma_sw = 0
        self.waited = {e: {} for e in self.ENGS}
        self.bar = {}
        self.pending_bar = {e: {} for e in self.ENGS}
        self.nflush = 0

    def add(self, eng, fn, reads=(), writes=(), dma=False):
        op = _Op()
        op.id = len(self.ops)
        op.eng = eng
        op.fn = fn
        op.dma = dma
        op.sig = False
        deps = set()
        for r in reads:
            w = self.last_w.get(r)
            if w is not None:
                deps.add(w)
        for k in writes:
            w = self.last_w.get(k)
            if w is not None:
                deps.add(w)
            for rd in self.readers.get(k, ()):
                deps.add(rd)
        deps.discard(op.id)
        op.deps = deps
        for r in reads:
            self.readers.setdefault(r, []).append(op.id)
        for k in writes:
            self.last_w[k] = op.id
            self.readers[k] = []
        self.ops.append(op)
        return op

    def flush(self, final=False):
        nc = self.nc
        import os
        mx = int(os.environ.get("MAXOPS", "0"))
        if mx and self.nflush >= 1 and (not os.environ.get("MAXOPS_ONLYH") or getattr(self, "in_H", False)):
            self.ops = self.ops[:mx]
        ops = self.ops
        if not ops:
            return
        for op in ops:
            for d in op.deps:
                dop = ops[d]
                if (not dop.dma) and (dop.eng != op.eng or op.eng != "pe"):
                    dop.sig = True
        last = {}
        for op in ops:
            if not op.dma:
                last[op.eng] = op
        for op in last.values():
            op.sig = True
        for op in ops:
            if op.dma:
                half = N_DMA_SEMS // 2
                if op.eng == "pool":
                    s = half + self.ndma_sw % half
                    self.ndma_sw += 1
                else:
                    s = self.ndma % half
                    self.ndma += 1
                op.sem = s
                op.prev_val = 16 * self.dma_use[s]
                self.dma_use[s] += 1
                op.val = 16 * self.dma_use[s]
            elif op.sig:
                self.cnt[op.eng] += 1
                op.val = self.cnt[op.eng]
        per = {e: [op for op in ops if op.eng == e] for e in self.ENGS}
        esem, dsem = self.esem, self.dsem

        def run(e, eng):
            waited = self.waited[e]
            first = True
            for op in per[e]:
                need = {}
                if first:
                    need.update(self.pending_bar[e])
                    self.pending_bar[e] = {}
                    first = False
                for d in op.deps:
                    dop = ops[d]
                    if dop.dma:
                        key = ("d", dop.sem)
                    else:
                        if dop.eng == e and e == "pe":
                            continue
                        key = ("e", dop.eng)
                    if dop.val > need.get(key, 0):
                        need[key] = dop.val
                if op.dma and op.prev_val > 0:
                    key = ("d", op.sem)
                    if op.prev_val > need.get(key, 0):
                        need[key] = op.prev_val
                for key, v in need.items():
                    if waited.get(key, 0) >= v:
                        continue
                    waited[key] = v
                    sem = dsem[key[1]] if key[0] == "d" else esem[key[1]]
                    eng.wait_ge(sem, v)
                ins = op.fn(eng)
                if op.dma:
                    ins.then_inc(dsem[op.sem], 16)
                elif op.sig:
                    ins.then_inc(esem[e], 1)
            if e == "sp":
                for s_ in range(N_DMA_SEMS):
                    v = 16 * self.dma_use[s_]
                    if v > waited.get(("d", s_), 0):
                        eng.wait_ge(dsem[s_], v)
                        waited[("d", s_)] = v

        with nc.Block() as block:
            @block.tensor
            def _(eng):
                run("pe", eng)

            @block.scalar
            def _(eng):
                run("act", eng)

            @block.vector
            def _(eng):
                run("dve", eng)

            @block.gpsimd
            def _(eng):
                run("pool", eng)

            @block.sync
            def _(eng):
                run("sp", eng)

        import os as _os
        if not final and _os.environ.get("SEMRESET"):
            with nc.Block() as block:
                @block.sync
                def _(eng):
                    for sm in list(self.esem.values()) + self.dsem:
                        eng.sem_clear(sm)
            self.cnt = {e: 0 for e in self.ENGS}
            self.dma_use = [0] * N_DMA_SEMS
            self.waited = {e: {} for e in self.ENGS}
        self.ops = []
        self.last_w = {}
        self.readers = {}
        self.nflush += 1


class K:
    def __init__(self, nc, P):
        self.nc = nc
        self.P = P
        self.dma_rr = 0

    def dma(self, out, in_, r=(), w=(), eng="sp", slow=False):
        if slow:
            return self.P.add(eng, lambda e: e.dma_start(out=out, in_=in_, allow_slow_non_contiguous=True), reads=r, writes=w, dma=True)
        return self.P.add(eng, lambda e: e.dma_start(out=out, in_=in_), reads=r, writes=w, dma=True)

    def mm(self, out, lhsT, rhs, start, stop, r=(), w=()):
        return self.P.add("pe", lambda e: e.matmul(out, lhsT=lhsT, rhs=rhs, start=start, stop=stop), reads=r, writes=w)

    def tr(self, out, in_, ident, r=(), w=()):
        return self.P.add("pe", lambda e: e.transpose(out=out, in_=in_, identity=ident), reads=r, writes=w)

    def act(self, out, in_, func, r=(), w=(), bias=None, scale=1.0, accum_out=None):
        kw = {}
        if bias is not None:
            kw["bias"] = bias
        if accum_out is not None:
            kw["accum_out"] = accum_out
        return self.P.add("act", lambda e: e.activation(out=out, in_=in_, func=func, scale=scale, **kw), reads=r, writes=w)

    def ts(self, eng, out, in0, s1, s2, op0, op1=None, r=(), w=()):
        if op1 is None:
            return self.P.add(eng, lambda e: e.tensor_scalar(out=out, in0=in0, scalar1=s1, scalar2=None, op0=op0), reads=r, writes=w)
        return self.P.add(eng, lambda e: e.tensor_scalar(out=out, in0=in0, scalar1=s1, scalar2=s2, op0=op0, op1=op1), reads=r, writes=w)

    def tt(self, eng, out, in0, in1, op, r=(), w=()):
        return self.P.add(eng, lambda e: e.tensor_tensor(out=out, in0=in0, in1=in1, op=op), reads=r, writes=w)

    def stt(self, eng, out, in0, scalar, in1, op0, op1, r=(), w=()):
        return self.P.add(eng, lambda e: e.scalar_tensor_tensor(out=out, in0=in0, scalar=scalar, in1=in1, op0=op0, op1=op1), reads=r, writes=w)

    def cp(self, eng, out, in_, r=(), w=()):
        if eng == "act":
            return self.P.add("act", lambda e: e.activation(out=out, in_=in_, func=AF.Identity), reads=r, writes=w)
        return self.P.add(eng, lambda e: e.tensor_copy(out=out, in_=in_), reads=r, writes=w)

    def memset(self, eng, out, val, w=()):
        return self.P.add(eng, lambda e: e.memset(out, val), writes=w)


def rmsnorm_tile(k, xt, gt, hn, scr, ms, tag):
    k.act(scr, xt, AF.Square, r=[tag + "x"], w=[tag + "scr", tag + "ms"], accum_out=ms)
    k.ts("dve", ms, ms, 1.0 / D, 1e-6, ALU.mult, ALU.add, r=[tag + "ms"], w=[tag + "ms"])
    k.act(ms, ms, AF.Sqrt, r=[tag + "ms"], w=[tag + "ms"])
    k.P.add("dve", lambda e: e.reciprocal(out=ms, in_=ms), reads=[tag + "ms"], writes=[tag + "ms"])
    k.stt("dve", hn, xt, ms, gt, ALU.mult, ALU.mult, r=[tag + "x", tag + "ms", "gt"], w=[tag + "hn"])


def build_program(dbg=None, stop_after=None):
    nc = bass.Bass("TRN2", target_bir_lowering=False)
    dt_in = {}

    def inp(name, shape, dt=F32):
        return nc.dram_tensor(name, list(shape), dt, kind="ExternalInput").ap()

    I = {}
    I["x"] = inp("x", [NTOK, D])
    for nm, shp in INPUT_SHAPES:
        I[nm] = inp(nm, shp)
    dbgset = set(dbg or ())
    out = nc.dram_tensor("out", [NTOK, D], F32, kind="ExternalOutput").ap()
    total = sum(int(np.prod(shp)) for _, shp in SCRATCH)
    big = nc.dram_tensor("scr", [total], F32, kind="ExternalOutput" if "scr" in dbgset else "Internal").ap()
    S = {}
    off = 0
    for nm, shp in SCRATCH:
        n = int(np.prod(shp))
        v = big[off:off + n]
        if len(shp) == 2:
            v = v.rearrange("(a b) -> a b", a=shp[0])
        elif len(shp) == 3:
            v = v.rearrange("(a b c) -> a b c", a=shp[0], b=shp[1])
        S[nm] = v
        off += n

    with contextlib.ExitStack() as top:
        P = Prog(nc, top)
        k = K(nc, P)
        ident_bf = top.enter_context(nc.sbuf_tensor("ident_bf", [128, 128], BF16))
        ident_f = top.enter_context(nc.sbuf_tensor("ident_f", [128, 128], F32))
        gt = top.enter_context(nc.sbuf_tensor("gt", [128, D], F32))
        k.memset("pool", ident_f[:], 1.0, w=["ident_f"])
        P.add("pool", lambda e: e.affine_select(out=ident_f[:], in_=ident_f[:], pattern=[[-1, 128]], compare_op=ALU.is_equal, fill=0.0, base=0, channel_multiplier=1), reads=["ident_f"], writes=["ident_f"])
        k.cp("dve", ident_bf[:], ident_f[:], r=["ident_f"], w=["ident_bf"])
        P.flush()
        C = dict(ident_bf=ident_bf, ident_f=ident_f, gt=gt)
        stop = stop_after
        import os
        if os.environ.get("ONLY_H"):
            stage_H(nc, k, P, I, S, C, out)
            P.flush(final=True)
            return nc
        stage_A(nc, k, P, I, S, C)
        if stop != "A":
            stage_B(nc, k, P, I, S, C)
        if stop not in ("A", "B"):
            stage_C(nc, k, P, I, S, C)
        if stop not in ("A", "B", "C"):
            stage_D(nc, k, P, I, S, C)
        if stop not in ("A", "B", "C", "D"):
            ffn_dense(nc, k, P, S, C, "E_", I["ffn_w_gate"], I["ffn_w_up"], I["ffn_w_down"], I["e_norm_ffn"][0, :], 22, out if stop == "E" else S["h"])
        if stop not in ("A", "B", "C", "D", "E"):
            stage_F1(nc, k, P, I, S, C)
        if stop not in ("A", "B", "C", "D", "E", "F1"):
            stage_F23(nc, k, P, I, S, C)
        if stop not in ("A", "B", "C", "D", "E", "F1", "F23"):
            stage_G(nc, k, P, I, S, C)
        if stop not in ("A", "B", "C", "D", "E", "F1", "F23", "G"):
            stage_H(nc, k, P, I, S, C, out)
        P.flush(final=True)
    return nc


SCRATCH = [
    ("pbT", [14, 128, NTOK]), ("mix0T", [8, 128, NTOK]), ("kapT", [128, 8, TP]), ("rsT", [128, 8, TP]),
    ("wT", [128, 8, TP]), ("nb_tok", [T, 8, 128]), ("k_tok", [T, 8, 128]), ("v_tok", [T, 8, 128]),
    ("r_tok", [T, 8, 128]), ("y_tok", [T, 8, 128]), ("sglT", [128, NTOK]), ("h", [NTOK, D]),
    ("qT", [4, 128, NTOK]), ("qrT", [4, 128, NTOK]), ("ksr", [2, 128, NTOK]), ("kwr", [2, 128, NTOK]),
    ("kcT", [128, NTOK]), ("vcT", [128, NTOK]), ("vs_tok", [NTOK, 128]), ("vw_tok", [NTOK, 128]),
    ("gates", [NTOK, 24]), ("mix1T", [8, 128, NTOK]),
]


def scratch_views(flat):
    out, off = {}, 0
    for nm, shp in SCRATCH:
        n = int(np.prod(shp))
        out[nm] = flat[off:off + n].reshape(shp)
        off += n
    return out


INPUT_SHAPES = [
    ("e_norm_mix", [1, D]), ("e_w_in", [D, 2816]), ("sgu_ln_g", [1, 512]), ("sgu_ln_b", [1, 512]),
    ("sgu_w", [4, 128, 128]), ("sgu_b", [4, 128]), ("rw_cols", [128, 30]),
    ("rwkv_w2", [64, 512]), ("rwkv_a2", [64, 512]), ("rwkv_g2", [128, 512]),
    ("rwkv_r_k", [1, 512]), ("rwkv_gn_g", [1, 512]), ("rwkv_gn_b", [1, 512]),
    ("e_w_out", [D, D]), ("e_norm_ffn", [1, D]), ("ffn_w_gate", [D, 2816]),
    ("ffn_w_up", [D, 2816]), ("ffn_w_down", [2816, D]),
    ("c_tril", [128, 128]), ("c_blk", [128, 128]), ("c_oh", [128, 32]),
    ("o_norm_mix", [1, D]), ("o_w_fm", [D, 3840]), ("o_w_tm", [D, 280]), ("c_cos", [128, T]), ("c_sin", [128, T]),
    ("conv_cols", [128, 12]),
    ("nsa_k_w1", [2048, 256]), ("nsa_v_w1", [2048, 256]), ("nsa_posk", [128, 32]), ("nsa_posv", [128, 32]),
    ("nsa_k_w2d", [256, 128]), ("nsa_v_w2", [256, 64]), ("c_ovl", [128, 32]), ("c_maskc", [128, 16, 128]),
    ("c_fbias", [128, 16, 32]), ("c_causal", [128, 128]), ("c_far", [128, 128]),
    ("o_w_out", [D, D]), ("o_norm_ffn", [1, D]), ("moe_router", [D, 8]), ("moe_router_b", [1, 8]),
    ("moe_w_gate", [8, D, 1408]), ("moe_w_up", [8, D, 1408]), ("moe_w_down", [8, 1408, D]), ("final_norm", [1, D]),
]


def host_inputs(inputs):
    f = lambda a: np.ascontiguousarray(np.asarray(a, dtype=np.float32))
    colT = lambda a, n: f(np.asarray(a).reshape(n, 128).T)
    m = {}
    m["e_norm_mix"] = f(inputs["e_norm_mix"])
    m["e_w_in"] = f(inputs["e_w_in"][0])
    m["sgu_ln_g"] = f(inputs["sgu_ln_g"])
    m["sgu_ln_b"] = f(inputs["sgu_ln_b"])
    m["sgu_w"] = f(inputs["sgu_w"][0])
    m["sgu_b"] = f(inputs["sgu_b"][0])
    m["rw_cols"] = f(np.concatenate([colT(inputs["rwkv_mu"][0], 14), colT(inputs["rwkv_w0"][0], 4), colT(inputs["rwkv_a0"][0], 4),
                                     colT(inputs["rwkv_k_k"][0], 4), colT(inputs["rwkv_k_a"][0], 4)], axis=1))
    m["rwkv_w2"] = f(inputs["rwkv_w2"][0])
    m["rwkv_a2"] = f(inputs["rwkv_a2"][0])
    m["rwkv_g2"] = f(inputs["rwkv_g2"][0])
    m["rwkv_r_k"] = f(np.asarray(inputs["rwkv_r_k"]).reshape(1, 512))
    m["rwkv_gn_g"] = f(inputs["rwkv_gn_g"])
    m["rwkv_gn_b"] = f(inputs["rwkv_gn_b"])
    m["e_w_out"] = f(inputs["e_w_out"][0])
    m["e_norm_ffn"] = f(inputs["e_norm_ffn"])
    m["ffn_w_gate"] = f(inputs["ffn_w_gate"][0])
    m["ffn_w_up"] = f(inputs["ffn_w_up"][0])
    m["ffn_w_down"] = f(inputs["ffn_w_down"][0])
    m["o_norm_mix"] = f(inputs["o_norm_mix"])
    owi = np.asarray(inputs["o_w_in"][0], dtype=np.float32)
    perm = np.arange(64)
    perm[:8] = np.arange(8, 16)
    perm[8:16] = np.arange(0, 8)
    Q0, KC, VC, KS, VS, KW, VW, GL, BG, CG, HD = 0, 512, 640, 768, 896, 1024, 1152, 1280, 1304, 1816, 2328
    cols = []
    cols += list(range(Q0, Q0 + 512))
    cols += [Q0 + hh * 64 + perm[d] for hh in range(8) for d in range(64)]
    dup = lambda base, pm: [base + hk * 64 + (perm[d] if pm else d) for hk in range(2) for _ in range(2) for d in range(64)]
    cols += dup(KS, False) + dup(KS, True) + dup(KW, False) + dup(KW, True)
    cols += list(range(KC, KC + 128)) + list(range(VC, VC + 128))
    cols += list(range(BG, BG + 512)) + list(range(CG, CG + 512)) + list(range(HD, HD + 512))
    m["o_w_fm"] = np.ascontiguousarray(owi[:, np.asarray(cols)])
    m["o_w_tm"] = np.ascontiguousarray(owi[:, list(range(VS, VS + 128)) + list(range(VW, VW + 128)) + list(range(GL, GL + 24))])
    m["conv_cols"] = f(np.asarray(inputs["conv_w"][0]).reshape(3, 4, 128).transpose(2, 1, 0).reshape(128, 12))
    m["o_w_out"] = f(inputs["o_w_out"][0])
    m["o_norm_ffn"] = f(inputs["o_norm_ffn"])
    m["moe_router"] = f(inputs["moe_router"][0])
    m["moe_router_b"] = f(inputs["moe_router_b"])
    m["moe_w_gate"] = f(inputs["moe_w_gate"][0])
    m["moe_w_up"] = f(inputs["moe_w_up"][0])
    m["moe_w_down"] = f(inputs["moe_w_down"][0])
    m["final_norm"] = f(np.asarray(inputs["final_norm"]).reshape(1, D))
    m["nsa_k_w1"] = f(inputs["nsa_cmp_k_w1"][0])
    m["nsa_v_w1"] = f(inputs["nsa_cmp_v_w1"][0])
    pk_ = np.asarray(inputs["nsa_cmp_pos_k"][0], dtype=np.float32).T
    pv_ = np.asarray(inputs["nsa_cmp_pos_v"][0], dtype=np.float32).T
    m["nsa_posk"] = np.ascontiguousarray(np.concatenate([pk_, pk_], 0))
    m["nsa_posv"] = np.ascontiguousarray(np.concatenate([pv_, pv_], 0))
    w2k_ = np.asarray(inputs["nsa_cmp_k_w2"][0], dtype=np.float32)
    m["nsa_k_w2d"] = np.ascontiguousarray(np.concatenate([w2k_, w2k_], 1))
    m["nsa_v_w2"] = f(inputs["nsa_cmp_v_w2"][0])
    NEG = np.float32(-1e30)
    pp_ = np.arange(128)
    n_ = np.arange(128)
    ovl = np.zeros((128, 32), np.float32)
    ci = np.arange(127)[:, None] * 16
    sj = np.arange(32)[None, :] * 64
    ovl[:127] = ((ci < sj + 64) & (ci + 32 > sj)).astype(np.float32)
    m["c_ovl"] = ovl
    mc = np.full((128, 16, 128), NEG, np.float32)
    fb = np.zeros((128, 16, 32), np.float32)
    for qb in range(16):
        tpos = qb * 128 + pp_
        ok = (16 * n_[None, :] + 31 <= tpos[:, None]) & (n_[None, :] < 127)
        mc[:, qb, :] = np.where(ok, np.float32(0), NEG)
        jb = np.arange(32)[None, :]
        cur = (tpos // 64)[:, None]
        forced = (jb == 0) | (jb == cur) | (jb == cur - 1)
        fb[:, qb, :] = np.where(jb * 64 <= tpos[:, None], np.where(forced, np.float32(1e6), np.float32(0)), NEG)
    m["c_maskc"] = mc
    m["c_fbias"] = fb
    m["c_causal"] = np.where(n_[None, :] <= pp_[:, None], np.float32(0), NEG).astype(np.float32)
    m["c_far"] = np.where(n_[None, :] > pp_[:, None], np.float32(0), NEG).astype(np.float32)
    posn = np.arange(T, dtype=np.float32)
    inv_freq = (500000.0 ** (-np.arange(0, 16, 2, dtype=np.float32) / 16)).astype(np.float32)
    ang = posn[None, :] * inv_freq[:, None]
    ct = np.ones((64, T), np.float32)
    stt_ = np.zeros((64, T), np.float32)
    ct[0:8] = np.cos(ang)
    ct[8:16] = np.cos(ang)
    stt_[0:8] = -np.sin(ang)
    stt_[8:16] = np.sin(ang)
    m["c_cos"] = np.ascontiguousarray(np.concatenate([ct, ct], 0))
    m["c_sin"] = np.ascontiguousarray(np.concatenate([stt_, stt_], 0))
    m["c_tril"] = np.tril(np.ones((128, 128), np.float32))
    blk = np.zeros((128, 128), np.float32)
    blk[:64, :64] = 1.0
    blk[64:, 64:] = 1.0
    m["c_blk"] = blk
    m["c_oh"] = (np.arange(128)[:, None] % 32 == np.arange(32)[None, :]).astype(np.float32)
    return m


def run(inputs, dbg=None, stop_after=None, ncores=NCORES):
    nc = build_program(dbg=dbg, stop_after=stop_after)
    m = host_inputs(inputs)
    x = np.asarray(inputs["x"], dtype=np.float32)
    in_maps = []
    for c in range(ncores):
        d = dict(m)
        d["x"] = np.ascontiguousarray(x[2 * c:2 * c + 2].reshape(NTOK, D))
        in_maps.append(d)
    res = run_bass_kernel_spmd(nc, in_maps, core_ids=list(range(ncores)))
    return res


def kernel(**inputs):
    res = run(inputs)
    outs = [r["out"].reshape(2, T, D) for r in res.results]
    return np.concatenate(outs, axis=0).astype(np.float32)


def load_cast(k, dst, src, ncols, r=(), w=(), step=1408):
    for c0 in range(0, ncols, step):
        c1 = min(ncols, c0 + step)
        k.dma(dst[:, :, c0:c1], src[:, :, c0:c1], r=r, w=w, eng="pool")


def stage_A(nc, k, P, I, S, C):
    ident_bf, gt = C["ident_bf"], C["gt"]
    with contextlib.ExitStack() as st:
        sb = lambda n, s, d: st.enter_context(nc.sbuf_tensor(n, s, d))
        ps = lambda n, s, d: st.enter_context(nc.psum_tensor(n, s, d))
        W = sb("A_W", [128, 8, 2816], BF16)
        lng = sb("A_lng", [128, 512], F32)
        lnb = sb("A_lnb", [128, 512], F32)
        wraw = sb("A_wraw", [128, 4, 128], F32)
        wmT = sb("A_wmT", [128, 4, 128], BF16)
        tril = sb("A_tril", [128, 128], F32)
        biasB = sb("A_biasB", [128, 4, 4, 128], F32)
        xt = [sb("A_xt%d" % i, [128, D], F32) for i in range(2)]
        scr = sb("A_scr", [128, D], F32)
        ms = [sb("A_ms%d" % i, [128, 1], F32) for i in range(2)]
        hn = [sb("A_hn%d" % i, [128, D], BF16) for i in range(2)]
        hnT = [sb("A_hnT%d" % i, [128, 8, 512], BF16) for i in range(2)]
        vsb = sb("A_v", [128, 512], F32)
        vsq = sb("A_vsq", [128, 512], F32)
        mean4 = sb("A_mean4", [128, 4], F32)
        var4 = sb("A_var4", [128, 4], F32)
        vn = sb("A_vn", [128, 4, 512], BF16)
        st6 = sb("A_st6", [128, 4, 6], F32)
        mv = sb("A_mv", [128, 4, 2], F32)
        uT = [sb("A_uT%d" % i, [128, 512], F32) for i in range(2)]
        yaT = [sb("A_yaT%d" % i, [128, 512], F32) for i in range(2)]
        pbs = [sb("A_pbs%d" % i, [128, 512], F32) for i in range(2)]
        tp = [ps("A_tp%d" % i, [128, 8, 128], BF16) for i in range(2)]
        pv = ps("A_pv", [128, 512], F32)
        pu = [ps("A_pu%d" % i, [128, 512], F32) for i in range(2)]
        pm = ps("A_pm", [128, 4, 128], F32)
        pp = [ps("A_pp%d" % i, [128, 512], F32) for i in range(2)]

        win = I["e_w_in"].rearrange("(k p) n -> p k n", p=128)
        load_cast(k, W, win, 2816, w=["W"])
        k.dma(gt[:], I["e_norm_mix"][0, :].partition_broadcast(128), w=["gt"])
        k.dma(lng[:], I["sgu_ln_g"][0, :].partition_broadcast(128), w=["lng"])
        k.dma(lnb[:], I["sgu_ln_b"][0, :].partition_broadcast(128), w=["lnb"])
        k.dma(tril[:], I["c_tril"], w=["tril"])
        k.dma(wraw[:], I["sgu_w"].rearrange("g t s -> t g s"), w=["wraw"])
        for g in range(4):
            for j in range(4):
                k.dma(biasB[:, g, j, :], I["sgu_b"][g, :].partition_broadcast(128), w=["biasB"])
        for g in range(4):
            k.tt("dve", wraw[:, g, :], wraw[:, g, :], tril[:], ALU.mult, r=["wraw", "tril"], w=["wraw"])
        pw = pp[0]
        for g in range(4):
            k.tr(pw[:, g * 128:(g + 1) * 128], wraw[:, g, :], C["ident_f"][:], r=["wraw", "ident_f"], w=["pp0"])
        k.cp("dve", wmT[:].rearrange("p g t -> p (g t)"), pw[:], r=["pp0"], w=["wmT"])

        for grp in range(8):
            hp = grp % 2
            hT = hnT[hp]
            for ti in range(4):
                tile = grp * 4 + ti
                b = tile % 2
                tagb = "A%d" % b
                k.dma(xt[b][:], I["x"][tile * 128:(tile + 1) * 128, :], w=[tagb + "x"])
                rmsnorm_tile(k, xt[b][:], gt[:], hn[b][:], scr[:], ms[b][:], tagb)
                for kk in range(8):
                    k.tr(tp[b][:, kk, :], hn[b][:, kk * 128:(kk + 1) * 128], ident_bf[:], r=[tagb + "hn", "ident_bf"], w=["tp%d" % b])
                k.cp("act", hT[:, :, ti * 128:(ti + 1) * 128], tp[b][:], r=["tp%d" % b], w=["hnT%d" % hp])
            for ti in range(4):
                for kk in range(8):
                    k.mm(pv[:], hT[:, kk, ti * 128:(ti + 1) * 128], W[:, kk, 512:1024], kk == 0, kk == 7, r=["hnT%d" % hp, "W"], w=["pv"])
                k.act(vsb[:], pv[:], AF.Gelu_apprx_tanh, r=["pv"], w=["vsb"])
                V4 = vsb[:].rearrange("p (g d) -> p g d", d=128)
                P.add("dve", lambda e, V4=V4: e.tensor_reduce(out=mean4[:], in_=V4, op=ALU.add, axis=AX.X), reads=["vsb"], writes=["mean4"])
                k.ts("dve", mean4[:], mean4[:], 1.0 / 128, None, ALU.mult, r=["mean4"], w=["mean4"])
                k.tt("dve", V4, V4, mean4[:].unsqueeze(2).broadcast_to([128, 4, 128]), ALU.subtract, r=["vsb", "mean4"], w=["vsb"])
                k.tt("dve", vsq[:], vsb[:], vsb[:], ALU.mult, r=["vsb"], w=["vsq"])
                P.add("dve", lambda e: e.tensor_reduce(out=var4[:], in_=vsq[:].rearrange("p (g d) -> p g d", d=128), op=ALU.add, axis=AX.X), reads=["vsq"], writes=["var4"])
                k.ts("dve", var4[:], var4[:], 1.0 / 128, 1e-5, ALU.mult, ALU.add, r=["var4"], w=["var4"])
                k.act(var4[:], var4[:], AF.Sqrt, r=["var4"], w=["var4"])
                P.add("dve", lambda e: e.reciprocal(out=var4[:], in_=var4[:]), reads=["var4"], writes=["var4"])
                k.tt("dve", V4, V4, var4[:].unsqueeze(2).broadcast_to([128, 4, 128]), ALU.mult, r=["vsb", "var4"], w=["vsb"])
                k.tt("dve", vsb[:], vsb[:], lng[:], ALU.mult, r=["vsb", "lng"], w=["vsb"])
                k.tt("dve", vn[:, ti, :], vsb[:], lnb[:], ALU.add, r=["vsb", "lnb"], w=["vn"])
                if C.get("dbgout") is not None and grp == 0:
                    k.dma(C["dbgout"][ti * 128:(ti + 1) * 128, 0:512], vsb[:], r=["vsb"])
                    k.dma(C["dbgout"][ti * 128:(ti + 1) * 128, 512:516], mean4[:], r=["mean4"])
                    k.dma(C["dbgout"][ti * 128:(ti + 1) * 128, 516:520], var4[:], r=["var4"])
            for g in range(4):
                ub = g % 2
                for kk in range(8):
                    k.mm(pu[ub][:], W[:, kk, g * 128:(g + 1) * 128], hT[:, kk, :], kk == 0, kk == 7, r=["hnT%d" % hp, "W"], w=["pu%d" % ub])
                k.act(uT[ub][:], pu[ub][:], AF.Gelu_apprx_tanh, r=["pu%d" % ub], w=["uT%d" % ub])
                for ti in range(4):
                    k.mm(pm[:, ti, :], vn[:, ti, g * 128:(g + 1) * 128], wmT[:, g, :], True, True, r=["vn", "wmT"], w=["pm"])
                k.tt("dve", pbs[0][:], pm[:].rearrange("p a b -> p (a b)"), biasB[:, g, :, :].rearrange("p a b -> p (a b)"), ALU.add, r=["pm", "biasB"], w=["pbs0"])
                k.tt("dve", yaT[ub][:], pbs[0][:], uT[ub][:], ALU.mult, r=["pbs0", "uT%d" % ub], w=["yaT%d" % ub])
                k.dma(S["mix0T"][g, :, grp * 512:(grp + 1) * 512], yaT[ub][:], r=["yaT%d" % ub], w=[])
            for c in range(14):
                cb = c % 2
                for kk in range(8):
                    k.mm(pp[cb][:], W[:, kk, 1024 + c * 128:1024 + (c + 1) * 128], hT[:, kk, :], kk == 0, kk == 7, r=["hnT%d" % hp, "W"], w=["pp%d" % cb])
                k.cp("dve" if c % 2 else "act", pbs[cb][:], pp[cb][:], r=["pp%d" % cb], w=["pbs%d" % cb])
                k.dma(S["pbT"][c, :, grp * 512:(grp + 1) * 512], pbs[cb][:], r=["pbs%d" % cb])
        P.flush()


def stage_B(nc, k, P, I, S, C):
    with contextlib.ExitStack() as st:
        sb = lambda n, s, d: st.enter_context(nc.sbuf_tensor(n, s, d))
        ps = lambda n, s, d: st.enter_context(nc.psum_tensor(n, s, d))
        cols = sb("B_cols", [128, 30], F32)
        omk = sb("B_omk", [128, 4], F32)
        w2b = sb("B_w2b", [128, 1, 512], BF16)
        a2b = sb("B_a2b", [128, 1, 512], BF16)
        blk = sb("B_blk", [128, 128], F32)
        cur = [sb("B_cur%d" % i, [128, 516], F32) for i in range(2)]
        tmp = sb("B_tmp", [128, 512], F32)
        XS = sb("B_XS", [128, 14, 512], F32)
        TW = sb("B_TW", [128, 512], BF16)
        sgl = sb("B_sgl", [128, 512], F32)
        sg = sb("B_sg", [128, 512], F32)
        dec = sb("B_dec", [128, 512], F32)
        av = sb("B_av", [128, 512], F32)
        KK = sb("B_KK", [128, 512], F32)
        sq = sb("B_sq", [128, 512], F32)
        nrm = sb("B_nrm", [128, 512], F32)
        kap = sb("B_kap", [128, 512], F32)
        t1 = sb("B_t1", [128, 512], F32)
        kp = sb("B_kp", [128, 512], F32)
        nb = sb("B_nb", [128, 512], F32)
        tk = [sb("B_tk%d" % i, [128, 4, 128], F32) for i in range(2)]
        pw = ps("B_pw", [128, 512], F32)
        pa = ps("B_pa", [128, 512], F32)
        pss = ps("B_pss", [128, 512], F32)
        ptr = [ps("B_ptr%d" % i, [128, 4, 128], F32) for i in range(2)]
        identf = C["ident_f"]

        k.dma(cols[:], I["rw_cols"], w=["cols"])
        k.dma(w2b[0:64, 0, :], I["rwkv_w2"], w=["w2b"], eng="pool")
        k.dma(a2b[64:128, 0, :], I["rwkv_a2"], w=["a2b"], eng="pool")
        k.dma(blk[:], I["c_blk"], w=["blk"])
        k.ts("dve", omk[:], cols[:, 26:30], -1.0, 1.0, ALU.mult, ALU.add, r=["cols"], w=["omk"])
        MU, W0, A0, KKc, KA = 0, 14, 18, 22, 26
        ntr = 0
        for b in range(2):
            for tg in range(4):
                g0 = b * T + tg * 512
                for c in range(14):
                    cb = c % 2
                    if tg == 0:
                        k.memset("pool", cur[cb][:, 0:1], 0.0, w=["cur%d" % cb])
                        k.dma(cur[cb][:, 1:513], S["pbT"][c, :, g0:g0 + 512], w=["cur%d" % cb])
                    else:
                        k.dma(cur[cb][:, 0:513], S["pbT"][c, :, g0 - 1:g0 + 512], w=["cur%d" % cb])
                    k.tt("dve", tmp[:], cur[cb][:, 0:512], cur[cb][:, 1:513], ALU.subtract, r=["cur%d" % cb], w=["tmp"])
                    k.stt("dve", XS[:, c, :], tmp[:], cols[:, MU + c:MU + c + 1], cur[cb][:, 1:513], ALU.mult, ALU.add, r=["tmp", "cols", "cur%d" % cb], w=["XS%d" % c])
                k.act(TW[0:64, :], XS[0:64, 12, :], AF.Tanh, r=["XS12"], w=["TW"])
                k.cp("act", TW[64:128, :], XS[64:128, 12, :], r=["XS12"], w=["TW"])
                k.act(sgl[:], XS[:, 13, :], AF.Sigmoid, r=["XS13"], w=["sgl"])
                k.dma(S["sglT"][:, g0:g0 + 512], sgl[:], r=["sgl"])
                for c in range(4):
                    g = b * 4 + c
                    tsl = slice(tg * 512, tg * 512 + 512)
                    k.mm(pw[:], w2b[0:64, 0, c * 128:(c + 1) * 128], TW[0:64, :], True, True, r=["w2b", "TW"], w=["pw"])
                    k.act(sg[:], pw[:], AF.Sigmoid, r=["pw", "cols"], w=["sg"], bias=cols[:, W0 + c:W0 + c + 1])
                    k.act(dec[:], sg[:], AF.Exp, r=["sg"], w=["dec"], scale=-math.exp(-0.5))
                    k.dma(S["wT"][:, g, tsl], dec[:], r=["dec"])
                    k.mm(pa[:], a2b[64:128, 0, c * 128:(c + 1) * 128], TW[64:128, :], True, True, r=["a2b", "TW"], w=["pa"])
                    k.act(av[:], pa[:], AF.Sigmoid, r=["pa", "cols"], w=["av"], bias=cols[:, A0 + c:A0 + c + 1])
                    k.ts("dve", KK[:], XS[:, 4 + c, :], cols[:, KKc + c:KKc + c + 1], None, ALU.mult, r=["XS%d" % (4 + c), "cols"], w=["KK"])
                    k.tt("pool", sq[:], KK[:], KK[:], ALU.mult, r=["KK"], w=["sq"])
                    k.mm(pss[:], blk[:], sq[:], True, True, r=["blk", "sq"], w=["pss"])
                    k.act(nrm[:], pss[:], AF.Sqrt, r=["pss"], w=["nrm"])
                    k.ts("dve", nrm[:], nrm[:], 1e-12, None, ALU.max, r=["nrm"], w=["nrm"])
                    P.add("dve", lambda e: e.reciprocal(out=nrm[:], in_=nrm[:]), reads=["nrm"], writes=["nrm"])
                    k.tt("dve", kap[:], KK[:], nrm[:], ALU.mult, r=["KK", "nrm"], w=["kap"])
                    k.dma(S["kapT"][:, g, tsl], kap[:], r=["kap"])
                    k.dma(S["rsT"][:, g, tg * 512 + 1:tg * 512 + 513], XS[:, c, :], r=["XS%d" % c])
                    k.ts("dve", t1[:], av[:], cols[:, KA + c:KA + c + 1], omk[:, c:c + 1], ALU.mult, ALU.add, r=["av", "cols", "omk"], w=["t1"])
                    k.tt("pool", kp[:], XS[:, 4 + c, :], t1[:], ALU.mult, r=["XS%d" % (4 + c), "t1"], w=["kp"])
                    k.stt("dve", nb[:], kap[:], -1.0, av[:], ALU.mult, ALU.mult, r=["kap", "av"], w=["nb"])
                    for (src, skey, dst) in ((nb[:], "nb", "nb_tok"), (kp[:], "kp", "k_tok"), (XS[:, 8 + c, :], "XS%d" % (8 + c), "v_tok"), (XS[:, c, :], "XS%d" % c, "r_tok")):
                        pb_ = ntr % 2
                        ntr += 1
                        for ti in range(4):
                            k.tr(ptr[pb_][:, ti, :], src[:, ti * 128:(ti + 1) * 128], identf[:], r=[skey, "ident_f"], w=["ptr%d" % pb_])
                        k.cp("act" if pb_ else "dve", tk[pb_][:], ptr[pb_][:], r=["ptr%d" % pb_], w=["tk%d" % pb_])
                        k.dma(S[dst][tg * 512:tg * 512 + 512, g, :].rearrange("(ti p) c -> p ti c", p=128), tk[pb_][:], r=["tk%d" % pb_])
        P.flush()


def stage_C(nc, k, P, I, S, C):
    with contextlib.ExitStack() as st:
        sb = lambda n, s, d: st.enter_context(nc.sbuf_tensor(n, s, d))
        ps = lambda n, s, d: st.enter_context(nc.psum_tensor(n, s, d))
        ZU = [sb("C_ZU%d" % i, [128, 8, 4, 32], BF16) for i in range(2)]
        WW = [sb("C_WW%d" % i, [128, 8, 32], F32) for i in range(2)]
        LBK = [sb("C_LBK%d" % i, [128, 8, 128], BF16) for i in range(2)]
        VW = [sb("C_VW%d" % i, [128, 8, 64], F32) for i in range(2)]
        oh = sb("C_oh", [128, 32], F32)
        Wexp = [sb("C_Wexp%d" % i, [128, 32, 8, 64], F32) for i in range(2)]
        M = sb("C_M", [128, 8, 64], F32)
        Mb = sb("C_Mb", [128, 8, 64], BF16)
        Mw = [sb("C_Mw%d" % i, [128, 8, 64], F32) for i in range(2)]
        UV = [sb("C_UV%d" % i, [128, 8, 64], BF16) for i in range(2)]
        Ya = [sb("C_Ya%d" % i, [128, 8, 64], F32) for i in range(2)]
        rfin = sb("C_rfin", [128, 8, 2], F32)
        yfin = sb("C_yfin", [2, 8, 64], F32)
        pu = [ps("C_pu%d" % i, [128, 8, 64], F32) for i in range(2)]
        pm = [ps("C_pm%d" % i, [128, 8, 64], F32) for i in range(2)]
        R = lambda ap: ap

        k.dma(oh[:], I["c_oh"], w=["oh"])
        k.memset("pool", M[:], 0.0, w=["M"])
        k.memset("pool", Mb[:], 0.0, w=["Mb"])
        for i in range(2):
            k.memset("pool", ZU[i][:], 0.0, w=["ZU%d" % i])
            k.memset("pool", LBK[i][:], 0.0, w=["LB%d" % i, "LK%d" % i])
        k.memset("pool", rfin[:], 0.0, w=["rfin"])
        NW = T // 32
        for w_ in range(NW):
            wp = w_ % 2
            t0 = w_ * 32
            zk, wk, lbk, lkk, vk, yk = "ZU%d" % wp, "WW%d" % wp, "LB%d" % wp, "LK%d" % wp, "VW%d" % wp, "Ya%d" % wp
            k.dma(ZU[wp][0:64, :, 0, :], S["kapT"][0:64, :, t0:t0 + 32], w=[zk], eng="pool")
            k.dma(ZU[wp][64:128, :, 1, :], S["kapT"][64:128, :, t0:t0 + 32], w=[zk], eng="pool")
            if w_ == 0:
                k.memset("pool", ZU[wp][:, :, 2:4, 0:1], 0.0, w=[zk])
                k.dma(ZU[wp][0:64, :, 2, 1:32], S["rsT"][0:64, :, 1:32], w=[zk], eng="pool")
                k.dma(ZU[wp][64:128, :, 3, 1:32], S["rsT"][64:128, :, 1:32], w=[zk], eng="pool")
            else:
                k.dma(ZU[wp][0:64, :, 2, :], S["rsT"][0:64, :, t0:t0 + 32], w=[zk], eng="pool")
                k.dma(ZU[wp][64:128, :, 3, :], S["rsT"][64:128, :, t0:t0 + 32], w=[zk], eng="pool")
            k.dma(WW[wp][:], S["wT"][:, :, t0:t0 + 32], w=[wk])
            k.dma(LBK[wp][0:32, :, 0:64], S["nb_tok"][t0:t0 + 32, :, 0:64], w=[lbk], eng="pool")
            k.dma(LBK[wp][32:64, :, 64:128], S["nb_tok"][t0:t0 + 32, :, 64:128], w=[lbk], eng="pool")
            k.dma(LBK[wp][64:96, :, 0:64], S["k_tok"][t0:t0 + 32, :, 0:64], w=[lkk], eng="pool")
            k.dma(LBK[wp][96:128, :, 64:128], S["k_tok"][t0:t0 + 32, :, 64:128], w=[lkk], eng="pool")
            k.dma(VW[wp][64:96, :, :], S["v_tok"][t0:t0 + 32, :, 0:64], w=[vk])
            k.dma(VW[wp][96:128, :, :], S["v_tok"][t0:t0 + 32, :, 64:128], w=[vk])
            k.act(Wexp[wp][:], WW[wp][:].rearrange("p g m -> p m g").unsqueeze(3).broadcast_to([128, 32, 8, 64]), AF.Identity, r=[wk], w=["Wexp%d" % wp])
            k.memset("pool", Ya[wp][64:128, :, :], 0.0, w=[yk])
            for m in range(32):
                sp_ = m % 2
                k.tt("dve", Mw[sp_][:], M[:], Wexp[wp][:, m, :, :], ALU.mult, r=["M", "Wexp%d" % wp], w=["Mw%d" % sp_])
                k.act(UV[sp_][64:128, :, :], VW[wp][64:128, :, :], AF.Identity, r=[vk, "oh"], w=["Vs%d" % sp_], scale=oh[64:128, m:m + 1])
                for g in range(8):
                    k.mm(pu[sp_][:, g, :], R(ZU[wp][:, g, :, :].rearrange("p a b -> p (a b)")), Mb[:, g, :], True, True, r=[zk, "Mb"], w=["pu%d" % sp_])
                k.act(UV[sp_][0:64, :, :], pu[sp_][0:64, :, :], AF.Identity, r=["pu%d" % sp_, "oh"], w=["Us%d" % sp_], scale=oh[0:64, m:m + 1])
                k.stt("dve", Ya[wp][64:128, :, :], pu[sp_][64:128, :, :], oh[64:128, m:m + 1], Ya[wp][64:128, :, :], ALU.mult, ALU.add, r=["pu%d" % sp_, "oh", yk], w=[yk])
                for g in range(8):
                    k.mm(pm[sp_][:, g, :], LBK[wp][:, g, :], UV[sp_][:, g, :], True, True, r=[lbk, lkk, "Us%d" % sp_, "Vs%d" % sp_], w=["pm%d" % sp_])
                k.tt("dve", Mb[:], Mw[sp_][:], pm[sp_][:], ALU.add, r=["Mw%d" % sp_, "pm%d" % sp_], w=["Mb"])
                k.tt("dve", M[:], Mw[sp_][:], pm[sp_][:], ALU.add, r=["Mw%d" % sp_, "pm%d" % sp_], w=["M"])
            lo = 1 if w_ == 0 else 0
            k.dma(S["y_tok"][t0 - 1 + lo:t0 + 31, :, 0:64], Ya[wp][64 + lo:96, :, :], r=[yk])
            k.dma(S["y_tok"][t0 - 1 + lo:t0 + 31, :, 64:128], Ya[wp][96 + lo:128, :, :], r=[yk])
        k.dma(rfin[0:64, :, 0:1], S["rsT"][0:64, :, T:T + 1], w=["rfin"], slow=True)
        k.dma(rfin[64:128, :, 1:2], S["rsT"][64:128, :, T:T + 1], w=["rfin"], slow=True)
        for g in range(8):
            k.mm(pu[0][0:2, g, :], rfin[:, g, :], M[:, g, :], True, True, r=["rfin", "M"], w=["pu0"])
        k.cp("dve", yfin[:], pu[0][0:2, :, :], r=["pu0"], w=["yfin"])
        k.dma(S["y_tok"][T - 1:T, :, 0:64], yfin[0:1, :, :], r=["yfin"])
        k.dma(S["y_tok"][T - 1:T, :, 64:128], yfin[1:2, :, :], r=["yfin"])
        P.flush()


def stage_D(nc, k, P, I, S, C):
    ident_bf = C["ident_bf"]
    with contextlib.ExitStack() as st:
        sb = lambda n, s, d: st.enter_context(nc.sbuf_tensor(n, s, d))
        ps = lambda n, s, d: st.enter_context(nc.psum_tensor(n, s, d))
        Wo = sb("D_Wo", [128, 8, 1024], BF16)
        g2b = sb("D_g2b", [128, 1, 512], BF16)
        gng = sb("D_gng", [128, 512], F32)
        gnb = sb("D_gnb", [128, 512], F32)
        rkb = sb("D_rkb", [128, 512], F32)
        yt = [sb("D_yt%d" % i, [128, 512], F32) for i in range(2)]
        rt = [sb("D_rt%d" % i, [128, 512], F32) for i in range(2)]
        kt = [sb("D_kt%d" % i, [128, 512], F32) for i in range(2)]
        vt = [sb("D_vt%d" % i, [128, 512], F32) for i in range(2)]
        xt = [sb("D_xt%d" % i, [128, D], F32) for i in range(2)]
        sgt = [sb("D_sgt%d" % i, [128, 128], BF16) for i in range(2)]
        mixT = [sb("D_mixT%d" % i, [128, 8, 128], BF16) for i in range(2)]
        ysq = sb("D_ysq", [128, 512], F32)
        s1 = sb("D_s1", [128, 8], F32)
        s2 = sb("D_s2", [128, 8], F32)
        m2 = sb("D_m2", [128, 8], F32)
        s3 = sb("D_s3", [128, 8], F32)
        yb = sb("D_yb", [128, 512], BF16)
        h1 = sb("D_h1", [128, D], F32)
        pg = ps("D_pg", [128, 512], F32)
        pt = ps("D_pt", [128, 4, 128], BF16)
        po = [ps("D_po%d" % i, [128, 512], F32) for i in range(2)]

        load_cast(k, Wo, I["e_w_out"].rearrange("(k p) n -> p k n", p=128), 1024, w=["Wo"], step=1024)
        k.dma(g2b[:, 0, :], I["rwkv_g2"], w=["g2b"], eng="pool")
        k.dma(gng[:], I["rwkv_gn_g"][0, :].partition_broadcast(128), w=["gng"])
        k.dma(gnb[:], I["rwkv_gn_b"][0, :].partition_broadcast(128), w=["gnb"])
        k.dma(rkb[:], I["rwkv_r_k"][0, :].partition_broadcast(128), w=["rkb"])
        V3 = lambda ap: ap.rearrange("p (h i) -> p h i", i=64)
        B3 = lambda ap: ap.unsqueeze(2).broadcast_to([128, 8, 64]) if hasattr(ap, "unsqueeze") else ap
        for tile in range(32):
            b, tt, pb_ = tile // 16, tile % 16, tile % 2
            tk_ = "D%d" % pb_
            sl = slice(tt * 128, tt * 128 + 128)
            gs = slice(b * 4, b * 4 + 4)
            k.dma(yt[pb_][:].rearrange("p (g c) -> p g c", c=128), S["y_tok"][sl, gs, :], w=[tk_ + "y"])
            k.dma(rt[pb_][:].rearrange("p (g c) -> p g c", c=128), S["r_tok"][sl, gs, :], w=[tk_ + "r"])
            k.dma(kt[pb_][:].rearrange("p (g c) -> p g c", c=128), S["k_tok"][sl, gs, :], w=[tk_ + "k"])
            k.dma(vt[pb_][:].rearrange("p (g c) -> p g c", c=128), S["v_tok"][sl, gs, :], w=[tk_ + "v"])
            k.dma(xt[pb_][:], I["x"][tile * 128:(tile + 1) * 128, :], w=[tk_ + "x"])
            k.dma(sgt[pb_][:], S["sglT"][:, tile * 128:(tile + 1) * 128], w=[tk_ + "sg"], eng="pool")
            k.dma(mixT[pb_][:, 0:4, :], S["mix0T"][0:4, :, tile * 128:(tile + 1) * 128].rearrange("c p t -> p c t"), w=[tk_ + "mix"], eng="pool")
            Y, Rr, Kk, Vv = yt[pb_], rt[pb_], kt[pb_], vt[pb_]
            P.add("dve", lambda e, Y=Y: e.tensor_reduce(out=s1[:], in_=V3(Y[:]), op=ALU.add, axis=AX.X), reads=[tk_ + "y"], writes=["s1"])
            k.tt("pool", ysq[:], Y[:], Y[:], ALU.mult, r=[tk_ + "y"], w=["ysq"])
            P.add("dve", lambda e: e.tensor_reduce(out=s2[:], in_=V3(ysq[:]), op=ALU.add, axis=AX.X), reads=["ysq"], writes=["s2"])
            k.ts("dve", s1[:], s1[:], 1.0 / 64, None, ALU.mult, r=["s1"], w=["s1"])
            k.tt("dve", m2[:], s1[:], s1[:], ALU.mult, r=["s1"], w=["m2"])
            k.stt("dve", s2[:], s2[:], 1.0 / 64, m2[:], ALU.mult, ALU.subtract, r=["s2", "m2"], w=["s2"])
            k.ts("dve", s2[:], s2[:], 64e-5, None, ALU.add, r=["s2"], w=["s2"])
            k.act(s2[:], s2[:], AF.Sqrt, r=["s2"], w=["s2"])
            P.add("dve", lambda e: e.reciprocal(out=s2[:], in_=s2[:]), reads=["s2"], writes=["s2"])
            k.tt("dve", V3(Y[:]), V3(Y[:]), s1[:].unsqueeze(2).broadcast_to([128, 8, 64]), ALU.subtract, r=[tk_ + "y", "s1"], w=[tk_ + "y"])
            k.tt("dve", V3(Y[:]), V3(Y[:]), s2[:].unsqueeze(2).broadcast_to([128, 8, 64]), ALU.mult, r=[tk_ + "y", "s2"], w=[tk_ + "y"])
            k.tt("dve", Y[:], Y[:], gng[:], ALU.mult, r=[tk_ + "y", "gng"], w=[tk_ + "y"])
            k.tt("dve", Y[:], Y[:], gnb[:], ALU.add, r=[tk_ + "y", "gnb"], w=[tk_ + "y"])
            k.tt("pool", Rr[:], Rr[:], Kk[:], ALU.mult, r=[tk_ + "r", tk_ + "k"], w=[tk_ + "r"])
            k.tt("pool", Rr[:], Rr[:], rkb[:], ALU.mult, r=[tk_ + "r", "rkb"], w=[tk_ + "r"])
            P.add("dve", lambda e, Rr=Rr: e.tensor_reduce(out=s3[:], in_=V3(Rr[:]), op=ALU.add, axis=AX.X), reads=[tk_ + "r"], writes=["s3"])
            k.tt("dve", V3(Vv[:]), V3(Vv[:]), s3[:].unsqueeze(2).broadcast_to([128, 8, 64]), ALU.mult, r=[tk_ + "v", "s3"], w=[tk_ + "v"])
            k.tt("dve", Y[:], Y[:], Vv[:], ALU.add, r=[tk_ + "y", tk_ + "v"], w=[tk_ + "y"])
            k.mm(pg[:], sgt[pb_][:], g2b[:, 0, :], True, True, r=[tk_ + "sg", "g2b"], w=["pg"])
            k.tt("dve", yb[:], Y[:], pg[:], ALU.mult, r=[tk_ + "y", "pg"], w=["yb"])
            for c in range(4):
                k.tr(pt[:, c, :], yb[:, c * 128:(c + 1) * 128], ident_bf[:], r=["yb", "ident_bf"], w=["pt"])
            k.cp("act", mixT[pb_][:, 4:8, :], pt[:], r=["pt"], w=[tk_ + "mix"])
            for hf in range(2):
                for c in range(8):
                    k.mm(po[hf][:], mixT[pb_][:, c, :], Wo[:, c, hf * 512:(hf + 1) * 512], c == 0, c == 7, r=[tk_ + "mix", "Wo"], w=["po%d" % hf])
                k.tt("dve", h1[:, hf * 512:(hf + 1) * 512], xt[pb_][:, hf * 512:(hf + 1) * 512], po[hf][:], ALU.add, r=[tk_ + "x", "po%d" % hf], w=["h1"])
            k.dma(S["h"][tile * 128:(tile + 1) * 128, :], h1[:], r=["h1"])
        P.flush()


def ffn_dense(nc, k, P, S, C, pre, Wg_d, Wu_d, Wd_d, gain_d, nf, dst):
    ident_bf, gt = C["ident_bf"], C["gt"]
    F = nf * 128
    with contextlib.ExitStack() as st:
        sb = lambda n, s, d: st.enter_context(nc.sbuf_tensor(n, s, d))
        ps = lambda n, s, d: st.enter_context(nc.psum_tensor(n, s, d))
        Wg = sb(pre + "Wg", [128, 8, F], BF16)
        Wu = sb(pre + "Wu", [128, 8, F], BF16)
        Wd = sb(pre + "Wd", [128, nf, 1024], BF16)
        h1s = sb(pre + "h1s", [128, 4, D], F32)
        junk = sb(pre + "junk", [128, D], BF16)
        ms = sb(pre + "ms", [128, 1], F32)
        hn = sb(pre + "hn", [128, D], BF16)
        hnT = sb(pre + "hnT", [128, 8, 512], BF16)
        actT = sb(pre + "actT", [128, nf, 512], BF16)
        sil = sb(pre + "sil", [128, 512], F32)
        ho = sb(pre + "ho", [128, 512], F32)
        tp = ps(pre + "tp", [128, 8, 128], BF16)
        pg = [ps(pre + "pg%d" % i, [128, 512], F32) for i in range(2)]
        pu = [ps(pre + "pu%d" % i, [128, 512], F32) for i in range(2)]
        pd = [ps(pre + "pd%d" % i, [128, 512], F32) for i in range(2)]
        load_cast(k, Wg, Wg_d.rearrange("(k p) n -> p k n", p=128), F, w=["Wg"])
        load_cast(k, Wu, Wu_d.rearrange("(k p) n -> p k n", p=128), F, w=["Wu"])
        load_cast(k, Wd, Wd_d.rearrange("(f p) n -> p f n", p=128), 1024, w=["Wd"], step=1024)
        k.dma(gt[:], gain_d.partition_broadcast(128), w=["gt"])
        for grp in range(8):
            for ti in range(4):
                tile = grp * 4 + ti
                k.dma(h1s[:, ti, :], S["h"][tile * 128:(tile + 1) * 128, :], w=["h1s%d" % ti])
                k.act(junk[:], h1s[:, ti, :], AF.Square, r=["h1s%d" % ti], w=["junk", "ms"], accum_out=ms[:])
                k.ts("dve", ms[:], ms[:], 1.0 / D, 1e-6, ALU.mult, ALU.add, r=["ms"], w=["ms"])
                k.act(ms[:], ms[:], AF.Sqrt, r=["ms"], w=["ms"])
                P.add("dve", lambda e: e.reciprocal(out=ms[:], in_=ms[:]), reads=["ms"], writes=["ms"])
                k.stt("dve", hn[:], h1s[:, ti, :], ms[:], gt[:], ALU.mult, ALU.mult, r=["h1s%d" % ti, "ms", "gt"], w=["hn"])
                for kk in range(8):
                    k.tr(tp[:, kk, :], hn[:, kk * 128:(kk + 1) * 128], ident_bf[:], r=["hn", "ident_bf"], w=["tp"])
                k.cp("act", hnT[:, :, ti * 128:(ti + 1) * 128], tp[:], r=["tp"], w=["hnT"])
            for f in range(nf):
                fb = f % 2
                for kk in range(8):
                    k.mm(pg[fb][:], Wg[:, kk, f * 128:(f + 1) * 128], hnT[:, kk, :], kk == 0, kk == 7, r=["Wg", "hnT"], w=["pg%d" % fb])
                for kk in range(8):
                    k.mm(pu[fb][:], Wu[:, kk, f * 128:(f + 1) * 128], hnT[:, kk, :], kk == 0, kk == 7, r=["Wu", "hnT"], w=["pu%d" % fb])
                k.act(sil[:], pg[fb][:], AF.Silu, r=["pg%d" % fb], w=["sil"])
                k.tt("dve", actT[:, f, :], sil[:], pu[fb][:], ALU.mult, r=["sil", "pu%d" % fb], w=["actT"])
            for ti in range(4):
                tile = grp * 4 + ti
                for hf in range(2):
                    for f in range(nf):
                        k.mm(pd[hf][:], actT[:, f, ti * 128:(ti + 1) * 128], Wd[:, f, hf * 512:(hf + 1) * 512], f == 0, f == nf - 1, r=["actT", "Wd"], w=["pd%d" % hf])
                    k.tt("dve", ho[:], h1s[:, ti, hf * 512:(hf + 1) * 512], pd[hf][:], ALU.add, r=["h1s%d" % ti, "pd%d" % hf], w=["ho"])
                    k.dma(dst[tile * 128:(tile + 1) * 128, hf * 512:(hf + 1) * 512], ho[:], r=["ho"], w=["hdst%d" % tile])
        P.flush()


def stage_F1(nc, k, P, I, S, C):
    ident_bf, gt = C["ident_bf"], C["gt"]
    NCH = 30
    with contextlib.ExitStack() as st:
        sb = lambda n, s, d: st.enter_context(nc.sbuf_tensor(n, s, d))
        ps = lambda n, s, d: st.enter_context(nc.psum_tensor(n, s, d))
        W = sb("F_W", [128, 8, NCH * 128], BF16)
        Wt = sb("F_Wt", [128, 8, 280], BF16)
        ctab = sb("F_ctab", [128, T], F32)
        stab = sb("F_stab", [128, T], F32)
        ccol = sb("F_ccol", [128, 12], F32)
        xt = [sb("F_xt%d" % i, [128, D], F32) for i in range(2)]
        scr = sb("F_scr", [128, D], BF16)
        ms = [sb("F_ms%d" % i, [128, 1], F32) for i in range(2)]
        hn = [sb("F_hn%d" % i, [128, D], BF16) for i in range(2)]
        hnT = sb("F_hnT", [128, 8, 512], BF16)
        FM = sb("F_FM", [128, NCH, 512], F32)
        tmp = sb("F_tmp", [128, 512], F32)
        Z = [sb("F_Z%d" % i, [128, 514], F32) for i in range(4)]
        ycv = sb("F_ycv", [128, 512], F32)
        tko = [sb("F_tko%d" % i, [128, 280], F32) for i in range(2)]
        tp = [ps("F_tp%d" % i, [128, 8, 128], BF16) for i in range(2)]
        pf = [ps("F_pf%d" % i, [128, 512], F32) for i in range(2)]
        pk = [ps("F_pk%d" % i, [128, 280], F32) for i in range(2)]
        load_cast(k, W, I["o_w_fm"].rearrange("(k p) n -> p k n", p=128), NCH * 128, w=["W"], step=1280)
        load_cast(k, Wt, I["o_w_tm"].rearrange("(k p) n -> p k n", p=128), 280, w=["Wt"], step=280)
        k.dma(ctab[:], I["c_cos"], w=["ctab"])
        k.dma(stab[:], I["c_sin"], w=["stab"])
        k.dma(ccol[:], I["conv_cols"], w=["ccol"])
        k.dma(gt[:], I["o_norm_mix"][0, :].partition_broadcast(128), w=["gt"])
        for grp in range(8):
            bq, tg = grp // 4, grp % 4
            ts_ = slice(tg * 512, tg * 512 + 512)
            gs_ = slice(grp * 512, grp * 512 + 512)
            for ti in range(4):
                tile = grp * 4 + ti
                b = tile % 2
                tagb = "F%d" % b
                k.dma(xt[b][:], S["h"][tile * 128:(tile + 1) * 128, :], w=[tagb + "x"])
                rmsnorm_tile(k, xt[b][:], gt[:], hn[b][:], scr[:], ms[b][:], tagb)
                for kk in range(8):
                    k.tr(tp[b][:, kk, :], hn[b][:, kk * 128:(kk + 1) * 128], ident_bf[:], r=[tagb + "hn", "ident_bf"], w=["tp%d" % b])
                k.cp("act", hnT[:, :, ti * 128:(ti + 1) * 128], tp[b][:], r=["tp%d" % b], w=["hnT"])
            for c in range(NCH):
                cb = c % 2
                for kk in range(8):
                    k.mm(pf[cb][:], W[:, kk, c * 128:(c + 1) * 128], hnT[:, kk, :], kk == 0, kk == 7, r=["hnT", "W"], w=["pf%d" % cb])
                k.cp("act" if cb else "dve", FM[:, c, :], pf[cb][:], r=["pf%d" % cb], w=["FM%d" % c])
            for ti in range(4):
                tile = grp * 4 + ti
                tb = ti % 2
                for kk in range(8):
                    k.mm(pk[tb][:], hnT[:, kk, ti * 128:(ti + 1) * 128], Wt[:, kk, :], kk == 0, kk == 7, r=["hnT", "Wt"], w=["pk%d" % tb])
                k.cp("dve", tko[tb][:, 0:256], pk[tb][:, 0:256], r=["pk%d" % tb], w=["tko%d" % tb])
                k.act(tko[tb][:, 256:280], pk[tb][:, 256:280], AF.Sigmoid, r=["pk%d" % tb], w=["tko%d" % tb])
                k.dma(S["vs_tok"][tile * 128:(tile + 1) * 128, :], tko[tb][:, 0:128], r=["tko%d" % tb])
                k.dma(S["vw_tok"][tile * 128:(tile + 1) * 128, :], tko[tb][:, 128:256], r=["tko%d" % tb])
                k.dma(S["gates"][tile * 128:(tile + 1) * 128, :], tko[tb][:, 256:280], r=["tko%d" % tb])
            def rope(cx, cp_, dst):
                k.tt("pool", tmp[:], FM[:, cp_, :], stab[:, ts_], ALU.mult, r=["FM%d" % cp_, "stab"], w=["tmp"])
                k.tt("dve", FM[:, cp_, :], FM[:, cx, :], ctab[:, ts_], ALU.mult, r=["FM%d" % cx, "ctab"], w=["FM%d" % cp_])
                k.tt("dve", FM[:, cp_, :], FM[:, cp_, :], tmp[:], ALU.add, r=["FM%d" % cp_, "tmp"], w=["FM%d" % cp_])
                k.dma(dst, FM[:, cp_, :], r=["FM%d" % cp_])
            for c in range(4):
                k.dma(S["qT"][c, :, gs_], FM[:, c, :], r=["FM%d" % c])
                rope(c, 4 + c, S["qrT"][c, :, gs_])
            for c in range(2):
                rope(8 + c, 10 + c, S["ksr"][c, :, gs_])
                rope(12 + c, 14 + c, S["kwr"][c, :, gs_])
            k.dma(S["kcT"][:, gs_], FM[:, 16, :], r=["FM16"])
            k.dma(S["vcT"][:, gs_], FM[:, 17, :], r=["FM17"])
            for c in range(4):
                zk = "Z%d" % c
                if tg == 0:
                    k.memset("pool", Z[c][:, 0:2], 0.0, w=[zk])
                else:
                    k.cp("pool", Z[c][:, 0:2], Z[c][:, 512:514], r=[zk], w=[zk])
                k.tt("dve", Z[c][:, 2:514], FM[:, 22 + c, :], FM[:, 26 + c, :], ALU.mult, r=["FM%d" % (22 + c), "FM%d" % (26 + c), zk], w=[zk])
                k.ts("dve", ycv[:], Z[c][:, 0:512], ccol[:, c * 3:c * 3 + 1], None, ALU.mult, r=[zk, "ccol"], w=["ycv"])
                k.stt("dve", ycv[:], Z[c][:, 1:513], ccol[:, c * 3 + 1:c * 3 + 2], ycv[:], ALU.mult, ALU.add, r=[zk, "ccol", "ycv"], w=["ycv"])
                k.stt("dve", ycv[:], Z[c][:, 2:514], ccol[:, c * 3 + 2:c * 3 + 3], ycv[:], ALU.mult, ALU.add, r=[zk, "ccol", "ycv"], w=["ycv"])
                k.tt("dve", ycv[:], ycv[:], FM[:, 18 + c, :], ALU.mult, r=["ycv", "FM%d" % (18 + c)], w=["ycv"])
                k.dma(S["mix1T"][4 + c, :, gs_], ycv[:], r=["ycv"])
        P.flush()


def stage_F23(nc, k, P, I, S, C):
    ident_bf = C["ident_bf"]
    SC = 0.125
    with contextlib.ExitStack() as st:
        sb = lambda n, s, d: st.enter_context(nc.sbuf_tensor(n, s, d))
        ps = lambda n, s, d: st.enter_context(nc.psum_tensor(n, s, d))
        w1 = [sb("G_w1%d" % i, [128, 32, 256], BF16) for i in range(2)]
        pos = [sb("G_pos%d" % i, [128, 32], BF16) for i in range(2)]
        w2k = sb("G_w2k", [128, 2, 128], BF16)
        w2v = sb("G_w2v", [128, 2, 64], BF16)
        maskc = sb("G_maskc", [128, 16, 128], F32)
        fbias = sb("G_fbias", [128, 16, 32], F32)
        ovl = sb("G_ovl", [128, 1, 32], BF16)
        causal = sb("G_causal", [128, 128], F32)
        far = sb("G_far", [128, 128], F32)
        cvb = [sb("G_cvb%d" % i, [128, 2048], BF16) for i in range(2)]
        biasc = sb("G_biasc", [128, 4], F32)
        gh = sb("G_gh", [128, 2, 128], BF16)
        kcmp = [[sb("G_kcmp%d%d" % (b, h), [128, 128], BF16) for h in range(2)] for b in range(2)]
        vcmp = [[sb("G_vcmp%d%d" % (b, h), [128, 64], BF16) for h in range(2)] for b in range(2)]
        ksr = [sb("G_ksr%d" % i, [128, 2048], BF16) for i in range(2)]
        kwr = [sb("G_kwr%d" % i, [128, 2048], BF16) for i in range(2)]
        vsb = sb("G_vs", [128, 16, 128], BF16)
        vwb = sb("G_vw", [128, 16, 128], BF16)
        qT = [sb("G_qT%d" % i, [128, 4, 128], BF16) for i in range(2)]
        qrT = [sb("G_qrT%d" % i, [128, 4, 128], BF16) for i in range(2)]
        gat = [sb("G_gat%d" % i, [128, 24], F32) for i in range(2)]
        ssb = [sb("G_ssb%d" % i, [128, 2048], F32) for i in range(4)]
        pexp = [sb("G_pexp%d" % i, [128, 2048], BF16) for i in range(4)]
        pTs = [sb("G_pTs%d" % i, [128, 16, 128], BF16) for i in range(4)]
        pn = [sb("G_pn%d" % i, [128, 128], BF16) for i in range(4)]
        st_m = sb("G_m", [128, 4], F32)
        st_s = sb("G_s", [128, 4], F32)
        rs3 = sb("G_rs3", [128, 3, 4], F32)
        coef = sb("G_coef", [128, 8], F32)
        impb = sb("G_impb", [128, 32], F32)
        top8 = sb("G_top8", [128, 8], F32)
        selb = sb("G_selb", [128, 32], F32)
        osb = sb("G_osb", [128, 3, 256], F32)
        yc = sb("G_yc", [128, 512], F32)
        ycb = sb("G_ycb", [128, 512], BF16)
        ycT = sb("G_ycT", [128, 4, 128], F32)
        pS = [ps("G_pS%d" % i, [128, 512], F32) for i in range(2)]
        pT = [ps("G_pT%d" % i, [128, 8, 128], BF16) for i in range(2)]
        pO = ps("G_pO", [128, 3, 256], F32)
        pI = ps("G_pI", [128, 32], F32)

        for i, nm in enumerate(("nsa_k_w1", "nsa_v_w1")):
            src = I[nm].rearrange("(l d) c -> d l c", d=64)
            k.dma(w1[i][0:64, :, :], src, w=["w1%d" % i], eng="pool")
            k.dma(w1[i][64:128, :, :], src, w=["w1%d" % i], eng="pool")
        k.dma(pos[0][:], I["nsa_posk"], w=["pos0"], eng="pool")
        k.dma(pos[1][:], I["nsa_posv"], w=["pos1"], eng="pool")
        k.dma(w2k[:], I["nsa_k_w2d"].rearrange("(cc p) d -> p cc d", p=128), w=["w2k"], eng="pool")
        k.dma(w2v[:], I["nsa_v_w2"].rearrange("(cc p) d -> p cc d", p=128), w=["w2v"], eng="pool")
        k.dma(ovl[:, 0, :], I["c_ovl"], w=["ovl"], eng="pool")
        k.dma(maskc[:], I["c_maskc"], w=["maskc"])
        k.dma(fbias[:], I["c_fbias"], w=["fbias"])
        k.dma(causal[:], I["c_causal"], w=["causal"])
        k.dma(far[:], I["c_far"], w=["far"])
        k.memset("pool", gh[:], 0.0, w=["gh"])
        for b in range(2):
            for h in range(2):
                k.memset("pool", kcmp[b][h][:], 0.0, w=["kcmp%d%d" % (b, h)])
                k.memset("pool", vcmp[b][h][:], 0.0, w=["vcmp%d%d" % (b, h)])
        pB = pI
        for kv in range(2):
            for cc in range(2):
                j = kv * 2 + cc
                for l in range(32):
                    k.mm(pB[:, j:j + 1], w1[kv][0:64, l, cc * 128:(cc + 1) * 128], pos[kv][0:64, l:l + 1], l == 0, l == 31, r=["w1%d" % kv, "pos%d" % kv], w=["pI"])
        k.cp("dve", biasc[:], pB[:, 0:4], r=["pI"], w=["biasc"])
        for b in range(2):
            bs = slice(b * T, (b + 1) * T)
            k.dma(cvb[0][:], S["kcT"][:, bs], w=["cvb0"], eng="pool")
            k.dma(cvb[1][:], S["vcT"][:, bs], w=["cvb1"], eng="pool")
            for hk in range(2):
                base = hk * 64
                for kv in range(2):
                    for cc in range(2):
                        for l in range(32):
                            k.mm(pS[cc][:, 0:127], w1[kv][base:base + 64, l, cc * 128:(cc + 1) * 128], cvb[kv][base:base + 64, l:l + 2017:16], l == 0, l == 31, r=["w1%d" % kv, "cvb%d" % kv], w=["pS%d" % cc])
                        k.act(gh[:, cc, 0:127], pS[cc][:, 0:127], AF.Gelu_apprx_tanh, r=["pS%d" % cc, "biasc"], w=["gh"], bias=biasc[:, kv * 2 + cc:kv * 2 + cc + 1])
                    if kv == 0:
                        for cc in range(2):
                            k.mm(pO[:, 0, 0:127], w2k[:, cc, :], gh[:, cc, 0:127], cc == 0, cc == 1, r=["w2k", "gh"], w=["pO"])
                        k.cp("dve", kcmp[b][hk][:, 0:127], pO[:, 0, 0:127], r=["pO"], w=["kcmp%d%d" % (b, hk)])
                    else:
                        for cc in range(2):
                            k.mm(pO[0:127, 1, 0:64], gh[:, cc, 0:127], w2v[:, cc, :], cc == 0, cc == 1, r=["w2v", "gh"], w=["pO"])
                        k.cp("dve", vcmp[b][hk][0:127, :], pO[0:127, 1, 0:64], r=["pO"], w=["vcmp%d%d" % (b, hk)])

        def softmax4(items):
            for g, src, dst, so in items:
                P.add("dve", lambda e, g=g, src=src: e.tensor_reduce(out=st_m[:, g:g + 1], in_=src, op=ALU.max, axis=AX.X), reads=["ssrc%d" % g], writes=["st_m%d" % g])
            for g, src, dst, so in items:
                k.ts("dve", st_m[:, g:g + 1], st_m[:, g:g + 1], -30000.0, -1.0, ALU.max, ALU.mult, r=["st_m%d" % g], w=["st_m%d" % g])
            for g, src, dst, so in items:
                k.act(dst, src, AF.Exp, r=["ssrc%d" % g, "st_m%d" % g], w=["pexp%d" % g, "st_s%d" % g], bias=st_m[:, g:g + 1], accum_out=st_s[:, g:g + 1])
            for g, src, dst, so in items:
                k.ts("dve", st_s[:, g:g + 1], st_s[:, g:g + 1], 1e-30, None, ALU.max, r=["st_s%d" % g], w=["st_s%d" % g])
            for g, src, dst, so in items:
                P.add("dve", lambda e, g=g, so=so: e.reciprocal(out=so, in_=st_s[:, g:g + 1]), reads=["st_s%d" % g], writes=["rs3_%d" % g])

        def transposes(g, n_tiles):
            for t0 in range(0, n_tiles, 8):
                pb_ = (t0 // 8) % 2
                nt = min(8, n_tiles - t0)
                for j in range(nt):
                    k.tr(pT[pb_][:, j, :], pexp[g][:, (t0 + j) * 128:(t0 + j + 1) * 128], ident_bf[:], r=["pexp%d" % g, "ident_bf"], w=["pT%d" % pb_])
                k.cp("act" if pb_ else "dve", pTs[g][:, t0:t0 + nt, :], pT[pb_][:, 0:nt, :], r=["pT%d" % pb_], w=["pTs%d" % g])

        nsb = 0
        for b in range(2):
            bs = slice(b * T, (b + 1) * T)
            for hk in range(2):
                k.dma(ksr[hk][:], S["ksr"][hk, :, bs], w=["ksr%d" % hk], eng="pool")
                k.dma(kwr[hk][:], S["kwr"][hk, :, bs], w=["kwr%d" % hk], eng="pool")
            k.dma(vsb[:], S["vs_tok"][bs, :].rearrange("(t p) c -> p t c", p=128), w=["vsb"], eng="pool")
            k.dma(vwb[:], S["vw_tok"][bs, :].rearrange("(t p) c -> p t c", p=128), w=["vwb"], eng="pool")
            for qb in range(16):
                qp = qb % 2
                tsl = slice(b * T + qb * 128, b * T + qb * 128 + 128)
                k.dma(qT[qp][:], S["qT"][:, :, tsl].rearrange("c p t -> p c t"), w=["qT%d" % qp], eng="pool")
                k.dma(qrT[qp][:], S["qrT"][:, :, tsl].rearrange("c p t -> p c t"), w=["qrT%d" % qp], eng="pool")
                k.dma(gat[qp][:], S["gates"][tsl, :], w=["gat%d" % qp])
                nkt = qb + 1
                wlo = max(0, qb - 4)
                nwt = qb - wlo + 1
                for hk in range(2):
                    hd = lambda g: ((hk * 4 + g) // 2, ((hk * 4 + g) % 2) * 64)
                    for g in range(4):
                        ch, pbs_ = hd(g)
                        sp_ = nsb % 2
                        nsb += 1
                        k.mm(pS[sp_][:, 0:128], qT[qp][pbs_:pbs_ + 64, ch, :], kcmp[b][hk][pbs_:pbs_ + 64, :], True, True, r=["qT%d" % qp, "kcmp%d%d" % (b, hk)], w=["pS%d" % sp_])
                        k.stt("dve", ssb[g][:, 0:128], pS[sp_][:, 0:128], SC, maskc[:, qb, :], ALU.mult, ALU.add, r=["pS%d" % sp_, "maskc"], w=["ssrc%d" % g])
                    softmax4([(g, ssb[g][:, 0:128], pexp[g][:, 0:128], rs3[:, 0, g:g + 1]) for g in range(4)])
                    for g in range(4):
                        k.ts("dve", pn[g][:], pexp[g][:, 0:128], rs3[:, 0, g:g + 1], None, ALU.mult, r=["pexp%d" % g, "rs3_%d" % g], w=["pn%d" % g])
                    for g in range(4):
                        k.tr(pT[0][:, g, :], pn[g][:], ident_bf[:], r=["pn%d" % g, "ident_bf"], w=["pT0"])
                    k.cp("act", pTs[0][:, 0:4, :], pT[0][:, 0:4, :], r=["pT0"], w=["pTs0"])
                    for g in range(4):
                        k.mm(pO[:, 0, g * 64:(g + 1) * 64], pTs[0][:, g, :], vcmp[b][hk][:], True, True, r=["pTs0", "vcmp%d%d" % (b, hk)], w=["pO"])
                    for g in range(4):
                        k.mm(pI[:], pTs[0][:, g, :], ovl[:, 0, :], g == 0, g == 3, r=["pTs0", "ovl"], w=["pI"])
                    k.tt("dve", impb[:], pI[:], fbias[:, qb, :], ALU.add, r=["pI", "fbias"], w=["impb"])
                    P.add("dve", lambda e: e.max(out=top8[:], in_=impb[:]), reads=["impb"], writes=["top8"])
                    k.ts("dve", selb[:], impb[:], top8[:, 7:8], None, ALU.is_ge, r=["impb", "top8"], w=["selb"])
                    k.ts("dve", selb[:], selb[:], 1e30, -1e30, ALU.mult, ALU.add, r=["selb"], w=["selb"])
                    nk = nkt * 128
                    for g in range(4):
                        ch, pbs_ = hd(g)
                        for k0 in range(0, nk, 512):
                            k1 = min(nk, k0 + 512)
                            sp_ = nsb % 2
                            nsb += 1
                            k.mm(pS[sp_][:, 0:k1 - k0], qrT[qp][pbs_:pbs_ + 64, ch, :], ksr[hk][pbs_:pbs_ + 64, k0:k1], True, True, r=["qrT%d" % qp, "ksr%d" % hk], w=["pS%d" % sp_])
                            nb_ = (k1 - k0) // 64
                            k.stt("dve", ssb[g][:, k0:k1].rearrange("p (j c) -> p j c", c=64), pS[sp_][:, 0:k1 - k0].rearrange("p (j c) -> p j c", c=64), SC,
                                  selb[:, k0 // 64:k0 // 64 + nb_].unsqueeze(2).broadcast_to([128, nb_, 64]), ALU.mult, ALU.add, r=["pS%d" % sp_, "selb"], w=["ssrc%d" % g])
                        k.tt("pool", ssb[g][:, qb * 128:(qb + 1) * 128], ssb[g][:, qb * 128:(qb + 1) * 128], causal[:], ALU.add, r=["ssrc%d" % g, "causal"], w=["ssrc%d" % g])
                    softmax4([(g, ssb[g][:, 0:nk], pexp[g][:, 0:nk], rs3[:, 1, g:g + 1]) for g in range(4)])
                    for g in range(4):
                        transposes(g, nkt)
                        for kt in range(nkt):
                            k.mm(pO[:, 1, g * 64:(g + 1) * 64], pTs[g][:, kt, :], vsb[:, kt, hk * 64:(hk + 1) * 64], kt == 0, kt == nkt - 1, r=["pTs%d" % g, "vsb"], w=["pO"])
                    nk = nwt * 128
                    kbase = wlo * 128
                    for g in range(4):
                        ch, pbs_ = hd(g)
                        for k0 in range(0, nk, 512):
                            k1 = min(nk, k0 + 512)
                            sp_ = nsb % 2
                            nsb += 1
                            k.mm(pS[sp_][:, 0:k1 - k0], qrT[qp][pbs_:pbs_ + 64, ch, :], kwr[hk][pbs_:pbs_ + 64, kbase + k0:kbase + k1], True, True, r=["qrT%d" % qp, "kwr%d" % hk], w=["pS%d" % sp_])
                            k.act(ssb[g][:, k0:k1], pS[sp_][:, 0:k1 - k0], AF.Identity, r=["pS%d" % sp_], w=["ssrc%d" % g], scale=SC)
                        if qb >= 4:
                            k.tt("pool", ssb[g][:, 0:128], ssb[g][:, 0:128], far[:], ALU.add, r=["ssrc%d" % g, "far"], w=["ssrc%d" % g])
                        k.tt("pool", ssb[g][:, nk - 128:nk], ssb[g][:, nk - 128:nk], causal[:], ALU.add, r=["ssrc%d" % g, "causal"], w=["ssrc%d" % g])
                    softmax4([(g, ssb[g][:, 0:nk], pexp[g][:, 0:nk], rs3[:, 2, g:g + 1]) for g in range(4)])
                    for g in range(4):
                        transposes(g, nwt)
                        for kt in range(nwt):
                            k.mm(pO[:, 2, g * 64:(g + 1) * 64], pTs[g][:, kt, :], vwb[:, wlo + kt, hk * 64:(hk + 1) * 64], kt == 0, kt == nwt - 1, r=["pTs%d" % g, "vwb"], w=["pO"])
                    k.cp("act", osb[:], pO[:], r=["pO"], w=["osb"])
                    rk = ["rs3_%d" % g for g in range(4)]
                    for g in range(4):
                        h = hk * 4 + g
                        dst = yc[:, h * 64:(h + 1) * 64]
                        k.ts("dve", dst, osb[:, 0, g * 64:(g + 1) * 64], gat[qp][:, h * 3:h * 3 + 1], None, ALU.mult, r=["osb", "gat%d" % qp], w=["yc"])
                        for br in (1, 2):
                            k.tt("dve", coef[:, g * 2 + br - 1:g * 2 + br], gat[qp][:, h * 3 + br:h * 3 + br + 1], rs3[:, br, g:g + 1], ALU.mult, r=["gat%d" % qp] + rk, w=["coef"])
                            k.stt("dve", dst, osb[:, br, g * 64:(g + 1) * 64], coef[:, g * 2 + br - 1:g * 2 + br], dst, ALU.mult, ALU.add, r=["osb", "coef", "yc"], w=["yc"])
                k.cp("dve", ycb[:], yc[:], r=["yc"], w=["ycb"])
                for c in range(4):
                    k.tr(pT[1][:, c, :], ycb[:, c * 128:(c + 1) * 128], ident_bf[:], r=["ycb", "ident_bf"], w=["pT1"])
                k.cp("act", ycT[:], pT[1][:, 0:4, :], r=["pT1"], w=["ycT"])
                k.dma(S["mix1T"][0:4, :, tsl].rearrange("c p t -> p c t"), ycT[:], r=["ycT"])
        P.flush()


def stage_G(nc, k, P, I, S, C):
    with contextlib.ExitStack() as st:
        sb = lambda n, s, d: st.enter_context(nc.sbuf_tensor(n, s, d))
        ps = lambda n, s, d: st.enter_context(nc.psum_tensor(n, s, d))
        Wo = sb("O_Wo", [128, 8, 1024], BF16)
        xt = [sb("O_xt%d" % i, [128, D], F32) for i in range(2)]
        mixT = [sb("O_mixT%d" % i, [128, 8, 128], BF16) for i in range(2)]
        h1 = [sb("O_h1%d" % i, [128, D], F32) for i in range(2)]
        po = [ps("O_po%d" % i, [128, 512], F32) for i in range(2)]
        load_cast(k, Wo, I["o_w_out"].rearrange("(k p) n -> p k n", p=128), 1024, w=["Wo"], step=1024)
        for tile in range(32):
            pb_ = tile % 2
            tk_ = "O%d" % pb_
            sl = slice(tile * 128, (tile + 1) * 128)
            k.dma(xt[pb_][:], S["h"][sl, :], w=[tk_ + "x"])
            k.dma(mixT[pb_][:], S["mix1T"][:, :, sl].rearrange("c p t -> p c t"), w=[tk_ + "mix"], eng="pool")
            for hf in range(2):
                for c in range(8):
                    k.mm(po[hf][:], mixT[pb_][:, c, :], Wo[:, c, hf * 512:(hf + 1) * 512], c == 0, c == 7, r=[tk_ + "mix", "Wo"], w=["po%d" % hf])
                k.tt("dve", h1[pb_][:, hf * 512:(hf + 1) * 512], xt[pb_][:, hf * 512:(hf + 1) * 512], po[hf][:], ALU.add, r=[tk_ + "x", "po%d" % hf], w=[tk_ + "h1"])
            k.dma(S["h"][sl, :], h1[pb_][:], r=[tk_ + "h1"])
        P.flush()


def stage_H(nc, k, P, I, S, C, out):
    ident_f, gt = C["ident_f"], C["gt"]
    P.in_H = True
    NF = 11
    with contextlib.ExitStack() as st:
        sb = lambda n, s, d: st.enter_context(nc.sbuf_tensor(n, s, d))
        ps = lambda n, s, d: st.enter_context(nc.psum_tensor(n, s, d))
        Wg = sb("H_Wg", [128, 8, 1408], BF16)
        Wu = sb("H_Wu", [128, 8, 1408], BF16)
        Wd = sb("H_Wd", [128, NF, 1024], BF16)
        wr = sb("H_wr", [128, 8, 8], F32)
        brt = sb("H_brt", [128, 8], F32)
        gfin = sb("H_gfin", [128, D], F32)
        import os
        nq = int(os.environ.get("H_NQ", "16"))
        acc = sb("H_acc", [128, nq, D], F32)
        hnT = sb("H_hnT", [128, 8, nq * 128], BF16)
        wgt = sb("H_wgt", [128, 16, 8], F32)
        hnf = sb("H_hnf", [128, D], F32)
        hnTf = sb("H_hnTf", [128, 8, 128], F32)
        junk = sb("H_junk", [128, D], BF16)
        ms = sb("H_ms", [128, 1], F32)
        lg = sb("H_lg", [128, 8], F32)
        top8 = sb("H_top8", [128, 8], F32)
        g12 = sb("H_g12", [128, 2], F32)
        eq = sb("H_eq", [128, 8], F32)
        actT = sb("H_actT", [128, NF, 512], BF16)
        sil = sb("H_sil", [128, 512], F32)
        ho = [sb("H_ho%d" % i, [128, D], F32) for i in range(2)]
        ptf = ps("H_ptf", [128, 8, 128], F32)
        pr = ps("H_pr", [128, 8], F32)
        pg = ps("H_pg", [128, 512], F32)
        pu = ps("H_pu", [128, 512], F32)
        pd = [ps("H_pd%d" % i, [128, 512], F32) for i in range(2)]
        k.dma(wr[:], I["moe_router"].rearrange("(k p) e -> p k e", p=128), w=["wr"])
        k.dma(brt[:], I["moe_router_b"][0, :].partition_broadcast(128), w=["brt"])
        k.dma(gt[:], I["o_norm_ffn"][0, :].partition_broadcast(128), w=["gt"])
        k.dma(gfin[:], I["final_norm"][0, :].partition_broadcast(128), w=["gfin"])
        import os
        for half in range(int(os.environ.get("H_HALVES", "2"))):
            for ti in range(int(os.environ.get("H_TILES", "16"))):
                tile = half * 16 + ti
                ak = "acc%d" % ti
                k.dma(acc[:, ti, :], S["h"][tile * 128:(tile + 1) * 128, :], w=[ak])
                k.act(junk[:], acc[:, ti, :], AF.Square, r=[ak], w=["junk", "ms"], accum_out=ms[:])
                k.ts("dve", ms[:], ms[:], 1.0 / D, 1e-6, ALU.mult, ALU.add, r=["ms"], w=["ms"])
                k.act(ms[:], ms[:], AF.Sqrt, r=["ms"], w=["ms"])
                P.add("dve", lambda e: e.reciprocal(out=ms[:], in_=ms[:]), reads=["ms"], writes=["ms"])
                k.stt("dve", hnf[:], acc[:, ti, :], ms[:], gt[:], ALU.mult, ALU.mult, r=[ak, "ms", "gt"], w=["hnf"])
                for kk in range(8):
                    k.tr(ptf[:, kk, :], hnf[:, kk * 128:(kk + 1) * 128], ident_f[:], r=["hnf", "ident_f"], w=["ptf"])
                for bk in range(2):
                    ks_ = slice(bk * 4, bk * 4 + 4)
                    k.cp("act", hnTf[:, ks_, :], ptf[:, ks_, :], r=["ptf"], w=["hnTf"])
                    k.cp("dve", hnT[:, ks_, ti * 128:(ti + 1) * 128], hnTf[:, ks_, :], r=["hnTf"], w=["hnT"])
                if os.environ.get("H_SKIPR"):
                    continue
                for kk in range(8):
                    k.mm(pr[:], hnTf[:, kk, :], wr[:, kk, :], kk == 0, kk == 7, r=["hnTf", "wr"], w=["pr"])
                k.tt("dve", lg[:], pr[:], brt[:], ALU.add, r=["pr", "brt"], w=["lg"])
                P.add("dve", lambda e: e.max(out=top8[:], in_=lg[:]), reads=["lg"], writes=["top8"])
                k.tt("dve", g12[:, 0:1], top8[:, 0:1], top8[:, 1:2], ALU.subtract, r=["top8"], w=["g12"])
                k.tt("dve", g12[:, 1:2], top8[:, 1:2], top8[:, 0:1], ALU.subtract, r=["top8"], w=["g12"])
                k.act(g12[:], g12[:], AF.Sigmoid, r=["g12"], w=["g12"])
                k.ts("dve", eq[:], lg[:], top8[:, 0:1], g12[:, 0:1], ALU.is_equal, ALU.mult, r=["lg", "top8", "g12"], w=["eq"])
                k.ts("dve", wgt[:, ti, :], lg[:], top8[:, 1:2], g12[:, 1:2], ALU.is_equal, ALU.mult, r=["lg", "top8", "g12"], w=["wgt"])
                k.tt("dve", wgt[:, ti, :], wgt[:, ti, :], eq[:], ALU.add, r=["wgt", "eq"], w=["wgt"])
            import os
            hphase = int(os.environ.get("H_PHASE", "3"))
            for e in range(int(os.environ.get("H_NE", "8")) if hphase >= 2 else 0):
                k.dma(Wg[:], I["moe_w_gate"][e].rearrange("(k p) n -> p k n", p=128), w=["Wg"], eng="pool")
                k.dma(Wu[:], I["moe_w_up"][e].rearrange("(k p) n -> p k n", p=128), w=["Wu"], eng="pool")
                k.dma(Wd[:], I["moe_w_down"][e].rearrange("(f p) n -> p f n", p=128), w=["Wd"], eng="pool")
                for grp in range(4):
                    gsl = slice(grp * 512, (grp + 1) * 512)
                    for f in range(NF):
                        for kk in range(8):
                            k.mm(pg[:], Wg[:, kk, f * 128:(f + 1) * 128], hnT[:, kk, gsl], kk == 0, kk == 7, r=["Wg", "hnT"], w=["pg"])
                        for kk in range(8):
                            k.mm(pu[:], Wu[:, kk, f * 128:(f + 1) * 128], hnT[:, kk, gsl], kk == 0, kk == 7, r=["Wu", "hnT"], w=["pu"])
                        k.act(sil[:], pg[:], AF.Silu, r=["pg"], w=["sil"])
                        k.tt("dve", actT[:, f, :], sil[:], pu[:], ALU.mult, r=["sil", "pu"], w=["actT"])
                    for t4 in range(4):
                        ti = grp * 4 + t4
                        ak = "acc%d" % ti
                        for hf in range(2):
                            for f in range(NF):
                                k.mm(pd[hf][:], actT[:, f, t4 * 128:(t4 + 1) * 128], Wd[:, f, hf * 512:(hf + 1) * 512], f == 0, f == NF - 1, r=["actT", "Wd"], w=["pd%d" % hf])
                            k.stt("dve", acc[:, ti, hf * 512:(hf + 1) * 512], pd[hf][:], wgt[:, ti, e:e + 1], acc[:, ti, hf * 512:(hf + 1) * 512], ALU.mult, ALU.add, r=["pd%d" % hf, "wgt", ak], w=[ak])
            for ti in range(16 if int(os.environ.get("H_P3", "1")) else 0):
                tile = half * 16 + ti
                ak = "acc%d" % ti
                hb = ti % 2
                k.act(junk[:], acc[:, ti, :], AF.Square, r=[ak], w=["junk", "ms"], accum_out=ms[:])
                k.ts("dve", ms[:], ms[:], 1.0 / D, 1e-6, ALU.mult, ALU.add, r=["ms"], w=["ms"])
                k.act(ms[:], ms[:], AF.Sqrt, r=["ms"], w=["ms"])
                P.add("dve", lambda e: e.reciprocal(out=ms[:], in_=ms[:]), reads=["ms"], writes=["ms"])
                k.stt("dve", ho[hb][:], acc[:, ti, :], ms[:], gfin[:], ALU.mult, ALU.mult, r=[ak, "ms", "gfin"], w=["ho%d" % hb])
                k.dma(out[tile * 128:(tile + 1) * 128, :], ho[hb][:], r=["ho%d" % hb])
        P.flush()
```

```python
import contextlib
import math
import numpy as np
import concourse.bass as bass
import concourse.mybir as mybir
from concourse.bass_utils import run_bass_kernel_spmd

F32 = mybir.dt.float32
F32R = mybir.dt.float32r
BF16 = mybir.dt.bfloat16
AF = mybir.ActivationFunctionType
ALU = mybir.AluOpType
AX = mybir.AxisListType

N_DMA_SEMS = 64
NCORES = 8
T = 2048
NTOK = 4096
D = 1024
TP = T + 64


class _Op:
    __slots__ = ("id", "eng", "fn", "deps", "dma", "sig", "sem", "val", "prev_val")


class Prog:
    ENGS = ("pe", "act", "dve", "pool", "sp")

    def __init__(self, nc, st):
        self.nc = nc
        self.ops = []
        self.last_w = {}
        self.readers = {}
        self.esem = {e: st.enter_context(nc.semaphore("s_" + e)) for e in self.ENGS}
        self.dsem = [st.enter_context(nc.semaphore("d_%d" % i)) for i in range(N_DMA_SEMS)]
        with nc.Block() as block:
            @block.sync
            def _(eng):
                for sm in list(self.esem.values()) + self.dsem:
                    eng.sem_clear(sm)
        self.cnt = {e: 0 for e in self.ENGS}
        self.dma_use = [0] * N_DMA_SEMS
        self.ndma = 0
        self.ndma_sw = 0
        self.waited = {e: {} for e in self.ENGS}
        self.bar = {}
        self.pending_bar = {e: {} for e in self.ENGS}
        self.nflush = 0

    def add(self, eng, fn, reads=(), writes=(), dma=False):
        op = _Op()
        op.id = len(self.ops)
        op.eng = eng
        op.fn = fn
        op.dma = dma
        op.sig = False
        deps = set()
        for r in reads:
            w = self.last_w.get(r)
            if w is not None:
                deps.add(w)
        for k in writes:
            w = self.last_w.get(k)
            if w is not None:
                deps.add(w)
            for rd in self.readers.get(k, ()):
                deps.add(rd)
        deps.discard(op.id)
        op.deps = deps
        for r in reads:
            self.readers.setdefault(r, []).append(op.id)
        for k in writes:
            self.last_w[k] = op.id
            self.readers[k] = []
        self.ops.append(op)
        return op

    def flush(self, final=False):
        nc = self.nc
        import os
        mx = int(os.environ.get("MAXOPS", "0"))
        if mx and self.nflush >= 1 and (not os.environ.get("MAXOPS_ONLYH") or getattr(self, "in_H", False)):
            self.ops = self.ops[:mx]
        ops = self.ops
        if not ops:
            return
        for op in ops:
            for d in op.deps:
                dop = ops[d]
                if (not dop.dma) and (dop.eng != op.eng or op.eng != "pe"):
                    dop.sig = True
        last = {}
        for op in ops:
            if not op.dma:
                last[op.eng] = op
        for op in last.values():
            op.sig = True
        for op in ops:
            if op.dma:
                half = N_DMA_SEMS // 2
                if op.eng == "pool":
                    s = half + self.ndma_sw % half
                    self.ndma_sw += 1
                else:
                    s = self.ndma % half
                    self.ndma += 1
                op.sem = s
                op.prev_val = 16 * self.dma_use[s]
                self.dma_use[s] += 1
                op.val = 16 * self.dma_use[s]
            elif op.sig:
                self.cnt[op.eng] += 1
                op.val = self.cnt[op.eng]
        per = {e: [op for op in ops if op.eng == e] for e in self.ENGS}
        esem, dsem = self.esem, self.dsem

        def run(e, eng):
            waited = self.waited[e]
            first = True
            for op in per[e]:
                need = {}
                if first:
                    need.update(self.pending_bar[e])
                    self.pending_bar[e] = {}
                    first = False
                for d in op.deps:
                    dop = ops[d]
                    if dop.dma:
                        key = ("d", dop.sem)
                    else:
                        if dop.eng == e and e == "pe":
                            continue
                        key = ("e", dop.eng)
                    if dop.val > need.get(key, 0):
                        need[key] = dop.val
                if op.dma and op.prev_val > 0:
                    key = ("d", op.sem)
                    if op.prev_val > need.get(key, 0):
                        need[key] = op.prev_val
                for key, v in need.items():
                    if waited.get(key, 0) >= v:
                        continue
                    waited[key] = v
                    sem = dsem[key[1]] if key[0] == "d" else esem[key[1]]
                    eng.wait_ge(sem, v)
                ins = op.fn(eng)
                if op.dma:
                    ins.then_inc(dsem[op.sem], 16)
                elif op.sig:
                    ins.then_inc(esem[e], 1)
            if e == "sp":
                for s_ in range(N_DMA_SEMS):
                    v = 16 * self.dma_use[s_]
                    if v > waited.get(("d", s_), 0):
                        eng.wait_ge(dsem[s_], v)
                        waited[("d", s_)] = v

        with nc.Block() as block:
            @block.tensor
            def _(eng):
                run("pe", eng)

            @block.scalar
            def _(eng):
                run("act", eng)

            @block.vector
            def _(eng):
                run("dve", eng)

            @block.gpsimd
            def _(eng):
                run("pool", eng)

            @block.sync
            def _(eng):
                run("sp", eng)

        import os as _os
        if not final and _os.environ.get("SEMRESET"):
            with nc.Block() as block:
                @block.sync
                def _(eng):
                    for sm in list(self.esem.values()) + self.dsem:
                        eng.sem_clear(sm)
            self.cnt = {e: 0 for e in self.ENGS}
            self.dma_use = [0] * N_DMA_SEMS
            self.waited = {e: {} for e in self.ENGS}
        self.ops = []
        self.last_w = {}
        self.readers = {}
        self.nflush += 1


class K:
    def __init__(self, nc, P):
        self.nc = nc
        self.P = P
        self.dma_rr = 0

    def dma(self, out, in_, r=(), w=(), eng="sp", slow=False):
        if slow:
            return self.P.add(eng, lambda e: e.dma_start(out=out, in_=in_, allow_slow_non_contiguous=True), reads=r, writes=w, dma=True)
        return self.P.add(eng, lambda e: e.dma_start(out=out, in_=in_), reads=r, writes=w, dma=True)

    def mm(self, out, lhsT, rhs, start, stop, r=(), w=()):
        return self.P.add("pe", lambda e: e.matmul(out, lhsT=lhsT, rhs=rhs, start=start, stop=stop), reads=r, writes=w)

    def tr(self, out, in_, ident, r=(), w=()):
        return self.P.add("pe", lambda e: e.transpose(out=out, in_=in_, identity=ident), reads=r, writes=w)

    def act(self, out, in_, func, r=(), w=(), bias=None, scale=1.0, accum_out=None):
        kw = {}
        if bias is not None:
            kw["bias"] = bias
        if accum_out is not None:
            kw["accum_out"] = accum_out
        return self.P.add("act", lambda e: e.activation(out=out, in_=in_, func=func, scale=scale, **kw), reads=r, writes=w)

    def ts(self, eng, out, in0, s1, s2, op0, op1=None, r=(), w=()):
        if op1 is None:
            return self.P.add(eng, lambda e: e.tensor_scalar(out=out, in0=in0, scalar1=s1, scalar2=None, op0=op0), reads=r, writes=w)
        return self.P.add(eng, lambda e: e.tensor_scalar(out=out, in0=in0, scalar1=s1, scalar2=s2, op0=op0, op1=op1), reads=r, writes=w)

    def tt(self, eng, out, in0, in1, op, r=(), w=()):
        return self.P.add(eng, lambda e: e.tensor_tensor(out=out, in0=in0, in1=in1, op=op), reads=r, writes=w)

    def stt(self, eng, out, in0, scalar, in1, op0, op1, r=(), w=()):
        return self.P.add(eng, lambda e: e.scalar_tensor_tensor(out=out, in0=in0, scalar=scalar, in1=in1, op0=op0, op1=op1), reads=r, writes=w)

    def cp(self, eng, out, in_, r=(), w=()):
        if eng == "act":
            return self.P.add("act", lambda e: e.activation(out=out, in_=in_, func=AF.Identity), reads=r, writes=w)
        return self.P.add(eng, lambda e: e.tensor_copy(out=out, in_=in_), reads=r, writes=w)

    def memset(self, eng, out, val, w=()):
        return self.P.add(eng, lambda e: e.memset(out, val), writes=w)


def rmsnorm_tile(k, xt, gt, hn, scr, ms, tag):
    k.act(scr, xt, AF.Square, r=[tag + "x"], w=[tag + "scr", tag + "ms"], accum_out=ms)
    k.ts("dve", ms, ms, 1.0 / D, 1e-6, ALU.mult, ALU.add, r=[tag + "ms"], w=[tag + "ms"])
    k.act(ms, ms, AF.Sqrt, r=[tag + "ms"], w=[tag + "ms"])
    k.P.add("dve", lambda e: e.reciprocal(out=ms, in_=ms), reads=[tag + "ms"], writes=[tag + "ms"])
    k.stt("dve", hn, xt, ms, gt, ALU.mult, ALU.mult, r=[tag + "x", tag + "ms", "gt"], w=[tag + "hn"])


def build_program(dbg=None, stop_after=None):
    nc = bass.Bass("TRN2", target_bir_lowering=False)
    dt_in = {}

    def inp(name, shape, dt=F32):
        return nc.dram_tensor(name, list(shape), dt, kind="ExternalInput").ap()

    I = {}
    I["x"] = inp("x", [NTOK, D])
    for nm, shp in INPUT_SHAPES:
        I[nm] = inp(nm, shp)
    dbgset = set(dbg or ())
    out = nc.dram_tensor("out", [NTOK, D], F32, kind="ExternalOutput").ap()
    total = sum(int(np.prod(shp)) for _, shp in SCRATCH)
    big = nc.dram_tensor("scr", [total], F32, kind="ExternalOutput" if "scr" in dbgset else "Internal").ap()
    S = {}
    off = 0
    for nm, shp in SCRATCH:
        n = int(np.prod(shp))
        v = big[off:off + n]
        if len(shp) == 2:
            v = v.rearrange("(a b) -> a b", a=shp[0])
        elif len(shp) == 3:
            v = v.rearrange("(a b c) -> a b c", a=shp[0], b=shp[1])
        S[nm] = v
        off += n

    with contextlib.ExitStack() as top:
        P = Prog(nc, top)
        k = K(nc, P)
        ident_bf = top.enter_context(nc.sbuf_tensor("ident_bf", [128, 128], BF16))
        ident_f = top.enter_context(nc.sbuf_tensor("ident_f", [128, 128], F32))
        gt = top.enter_context(nc.sbuf_tensor("gt", [128, D], F32))
        k.memset("pool", ident_f[:], 1.0, w=["ident_f"])
        P.add("pool", lambda e: e.affine_select(out=ident_f[:], in_=ident_f[:], pattern=[[-1, 128]], compare_op=ALU.is_equal, fill=0.0, base=0, channel_multiplier=1), reads=["ident_f"], writes=["ident_f"])
        k.cp("dve", ident_bf[:], ident_f[:], r=["ident_f"], w=["ident_bf"])
        P.flush()
        C = dict(ident_bf=ident_bf, ident_f=ident_f, gt=gt)
        stop = stop_after
        import os
        if os.environ.get("ONLY_H"):
            stage_H(nc, k, P, I, S, C, out)
            P.flush(final=True)
            return nc
        stage_A(nc, k, P, I, S, C)
        if stop != "A":
            stage_B(nc, k, P, I, S, C)
        if stop not in ("A", "B"):
            stage_C(nc, k, P, I, S, C)
        if stop not in ("A", "B", "C"):
            stage_D(nc, k, P, I, S, C)
        if stop not in ("A", "B", "C", "D"):
            ffn_dense(nc, k, P, S, C, "E_", I["ffn_w_gate"], I["ffn_w_up"], I["ffn_w_down"], I["e_norm_ffn"][0, :], 22, out if stop == "E" else S["h"])
        if stop not in ("A", "B", "C", "D", "E"):
            stage_F1(nc, k, P, I, S, C)
        if stop not in ("A", "B", "C", "D", "E", "F1"):
            stage_F23(nc, k, P, I, S, C)
        if stop not in ("A", "B", "C", "D", "E", "F1", "F23"):
            stage_G(nc, k, P, I, S, C)
        if stop not in ("A", "B", "C", "D", "E", "F1", "F23", "G"):
            stage_H(nc, k, P, I, S, C, out)
        P.flush(final=True)
    return nc


SCRATCH = [
    ("pbT", [14, 128, NTOK]), ("mix0T", [8, 128, NTOK]), ("kapT", [128, 8, TP]), ("rsT", [128, 8, TP]),
    ("wT", [128, 8, TP]), ("nb_tok", [T, 8, 128]), ("k_tok", [T, 8, 128]), ("v_tok", [T, 8, 128]),
    ("r_tok", [T, 8, 128]), ("y_tok", [T, 8, 128]), ("sglT", [128, NTOK]), ("h", [NTOK, D]),
    ("qT", [4, 128, NTOK]), ("qrT", [4, 128, NTOK]), ("ksr", [2, 128, NTOK]), ("kwr", [2, 128, NTOK]),
    ("kcT", [128, NTOK]), ("vcT", [128, NTOK]), ("vs_tok", [NTOK, 128]), ("vw_tok", [NTOK, 128]),
    ("gates", [NTOK, 24]), ("mix1T", [8, 128, NTOK]),
]


def scratch_views(flat):
    out, off = {}, 0
    for nm, shp in SCRATCH:
        n = int(np.prod(shp))
        out[nm] = flat[off:off + n].reshape(shp)
        off += n
    return out


INPUT_SHAPES = [
    ("e_norm_mix", [1, D]), ("e_w_in", [D, 2816]), ("sgu_ln_g", [1, 512]), ("sgu_ln_b", [1, 512]),
    ("sgu_w", [4, 128, 128]), ("sgu_b", [4, 128]), ("rw_cols", [128, 30]),
    ("rwkv_w2", [64, 512]), ("rwkv_a2", [64, 512]), ("rwkv_g2", [128, 512]),
    ("rwkv_r_k", [1, 512]), ("rwkv_gn_g", [1, 512]), ("rwkv_gn_b", [1, 512]),
    ("e_w_out", [D, D]), ("e_norm_ffn", [1, D]), ("ffn_w_gate", [D, 2816]),
    ("ffn_w_up", [D, 2816]), ("ffn_w_down", [2816, D]),
    ("c_tril", [128, 128]), ("c_blk", [128, 128]), ("c_oh", [128, 32]),
    ("o_norm_mix", [1, D]), ("o_w_fm", [D, 3840]), ("o_w_tm", [D, 280]), ("c_cos", [128, T]), ("c_sin", [128, T]),
    ("conv_cols", [128, 12]),
    ("nsa_k_w1", [2048, 256]), ("nsa_v_w1", [2048, 256]), ("nsa_posk", [128, 32]), ("nsa_posv", [128, 32]),
    ("nsa_k_w2d", [256, 128]), ("nsa_v_w2", [256, 64]), ("c_ovl", [128, 32]), ("c_maskc", [128, 16, 128]),
    ("c_fbias", [128, 16, 32]), ("c_causal", [128, 128]), ("c_far", [128, 128]),
    ("o_w_out", [D, D]), ("o_norm_ffn", [1, D]), ("moe_router", [D, 8]), ("moe_router_b", [1, 8]),
    ("moe_w_gate", [8, D, 1408]), ("moe_w_up", [8, D, 1408]), ("moe_w_down", [8, 1408, D]), ("final_norm", [1, D]),
]


def host_inputs(inputs):
    f = lambda a: np.ascontiguousarray(np.asarray(a, dtype=np.float32))
    colT = lambda a, n: f(np.asarray(a).reshape(n, 128).T)
    m = {}
    m["e_norm_mix"] = f(inputs["e_norm_mix"])
    m["e_w_in"] = f(inputs["e_w_in"][0])
    m["sgu_ln_g"] = f(inputs["sgu_ln_g"])
    m["sgu_ln_b"] = f(inputs["sgu_ln_b"])
    m["sgu_w"] = f(inputs["sgu_w"][0])
    m["sgu_b"] = f(inputs["sgu_b"][0])
    m["rw_cols"] = f(np.concatenate([colT(inputs["rwkv_mu"][0], 14), colT(inputs["rwkv_w0"][0], 4), colT(inputs["rwkv_a0"][0], 4),
                                     colT(inputs["rwkv_k_k"][0], 4), colT(inputs["rwkv_k_a"][0], 4)], axis=1))
    m["rwkv_w2"] = f(inputs["rwkv_w2"][0])
    m["rwkv_a2"] = f(inputs["rwkv_a2"][0])
    m["rwkv_g2"] = f(inputs["rwkv_g2"][0])
    m["rwkv_r_k"] = f(np.asarray(inputs["rwkv_r_k"]).reshape(1, 512))
    m["rwkv_gn_g"] = f(inputs["rwkv_gn_g"])
    m["rwkv_gn_b"] = f(inputs["rwkv_gn_b"])
    m["e_w_out"] = f(inputs["e_w_out"][0])
    m["e_norm_ffn"] = f(inputs["e_norm_ffn"])
    m["ffn_w_gate"] = f(inputs["ffn_w_gate"][0])
    m["ffn_w_up"] = f(inputs["ffn_w_up"][0])
    m["ffn_w_down"] = f(inputs["ffn_w_down"][0])
    m["o_norm_mix"] = f(inputs["o_norm_mix"])
    owi = np.asarray(inputs["o_w_in"][0], dtype=np.float32)
    perm = np.arange(64)
    perm[:8] = np.arange(8, 16)
    perm[8:16] = np.arange(0, 8)
    Q0, KC, VC, KS, VS, KW, VW, GL, BG, CG, HD = 0, 512, 640, 768, 896, 1024, 1152, 1280, 1304, 1816, 2328
    cols = []
    cols += list(range(Q0, Q0 + 512))
    cols += [Q0 + hh * 64 + perm[d] for hh in range(8) for d in range(64)]
    dup = lambda base, pm: [base + hk * 64 + (perm[d] if pm else d) for hk in range(2) for _ in range(2) for d in range(64)]
    cols += dup(KS, False) + dup(KS, True) + dup(KW, False) + dup(KW, True)
    cols += list(range(KC, KC + 128)) + list(range(VC, VC + 128))
    cols += list(range(BG, BG + 512)) + list(range(CG, CG + 512)) + list(range(HD, HD + 512))
    m["o_w_fm"] = np.ascontiguousarray(owi[:, np.asarray(cols)])
    m["o_w_tm"] = np.ascontiguousarray(owi[:, list(range(VS, VS + 128)) + list(range(VW, VW + 128)) + list(range(GL, GL + 24))])
    m["conv_cols"] = f(np.asarray(inputs["conv_w"][0]).reshape(3, 4, 128).transpose(2, 1, 0).reshape(128, 12))
    m["o_w_out"] = f(inputs["o_w_out"][0])
    m["o_norm_ffn"] = f(inputs["o_norm_ffn"])
    m["moe_router"] = f(inputs["moe_router"][0])
    m["moe_router_b"] = f(inputs["moe_router_b"])
    m["moe_w_gate"] = f(inputs["moe_w_gate"][0])
    m["moe_w_up"] = f(inputs["moe_w_up"][0])
    m["moe_w_down"] = f(inputs["moe_w_down"][0])
    m["final_norm"] = f(np.asarray(inputs["final_norm"]).reshape(1, D))
    m["nsa_k_w1"] = f(inputs["nsa_cmp_k_w1"][0])
    m["nsa_v_w1"] = f(inputs["nsa_cmp_v_w1"][0])
    pk_ = np.asarray(inputs["nsa_cmp_pos_k"][0], dtype=np.float32).T
    pv_ = np.asarray(inputs["nsa_cmp_pos_v"][0], dtype=np.float32).T
    m["nsa_posk"] = np.ascontiguousarray(np.concatenate([pk_, pk_], 0))
    m["nsa_posv"] = np.ascontiguousarray(np.concatenate([pv_, pv_], 0))
    w2k_ = np.asarray(inputs["nsa_cmp_k_w2"][0], dtype=np.float32)
    m["nsa_k_w2d"] = np.ascontiguousarray(np.concatenate([w2k_, w2k_], 1))
    m["nsa_v_w2"] = f(inputs["nsa_cmp_v_w2"][0])
    NEG = np.float32(-1e30)
    pp_ = np.arange(128)
    n_ = np.arange(128)
    ovl = np.zeros((128, 32), np.float32)
    ci = np.arange(127)[:, None] * 16
    sj = np.arange(32)[None, :] * 64
    ovl[:127] = ((ci < sj + 64) & (ci + 32 > sj)).astype(np.float32)
    m["c_ovl"] = ovl
    mc = np.full((128, 16, 128), NEG, np.float32)
    fb = np.zeros((128, 16, 32), np.float32)
    for qb in range(16):
        tpos = qb * 128 + pp_
        ok = (16 * n_[None, :] + 31 <= tpos[:, None]) & (n_[None, :] < 127)
        mc[:, qb, :] = np.where(ok, np.float32(0), NEG)
        jb = np.arange(32)[None, :]
        cur = (tpos // 64)[:, None]
        forced = (jb == 0) | (jb == cur) | (jb == cur - 1)
        fb[:, qb, :] = np.where(jb * 64 <= tpos[:, None], np.where(forced, np.float32(1e6), np.float32(0)), NEG)
    m["c_maskc"] = mc
    m["c_fbias"] = fb
    m["c_causal"] = np.where(n_[None, :] <= pp_[:, None], np.float32(0), NEG).astype(np.float32)
    m["c_far"] = np.where(n_[None, :] > pp_[:, None], np.float32(0), NEG).astype(np.float32)
    posn = np.arange(T, dtype=np.float32)
    inv_freq = (500000.0 ** (-np.arange(0, 16, 2, dtype=np.float32) / 16)).astype(np.float32)
    ang = posn[None, :] * inv_freq[:, None]
    ct = np.ones((64, T), np.float32)
    stt_ = np.zeros((64, T), np.float32)
    ct[0:8] = np.cos(ang)
    ct[8:16] = np.cos(ang)
    stt_[0:8] = -np.sin(ang)
    stt_[8:16] = np.sin(ang)
    m["c_cos"] = np.ascontiguousarray(np.concatenate([ct, ct], 0))
    m["c_sin"] = np.ascontiguousarray(np.concatenate([stt_, stt_], 0))
    m["c_tril"] = np.tril(np.ones((128, 128), np.float32))
    blk = np.zeros((128, 128), np.float32)
    blk[:64, :64] = 1.0
    blk[64:, 64:] = 1.0
    m["c_blk"] = blk
    m["c_oh"] = (np.arange(128)[:, None] % 32 == np.arange(32)[None, :]).astype(np.float32)
    return m


def run(inputs, dbg=None, stop_after=None, ncores=NCORES):
    nc = build_program(dbg=dbg, stop_after=stop_after)
    m = host_inputs(inputs)
    x = np.asarray(inputs["x"], dtype=np.float32)
    in_maps = []
    for c in range(ncores):
        d = dict(m)
        d["x"] = np.ascontiguousarray(x[2 * c:2 * c + 2].reshape(NTOK, D))
        in_maps.append(d)
    res = run_bass_kernel_spmd(nc, in_maps, core_ids=list(range(ncores)))
    return res


def kernel(**inputs):
    res = run(inputs)
    outs = [r["out"].reshape(2, T, D) for r in res.results]
    return np.concatenate(outs, axis=0).astype(np.float32)


def load_cast(k, dst, src, ncols, r=(), w=(), step=1408):
    for c0 in range(0, ncols, step):
        c1 = min(ncols, c0 + step)
        k.dma(dst[:, :, c0:c1], src[:, :, c0:c1], r=r, w=w, eng="pool")


def stage_A(nc, k, P, I, S, C):
    ident_bf, gt = C["ident_bf"], C["gt"]
    with contextlib.ExitStack() as st:
        sb = lambda n, s, d: st.enter_context(nc.sbuf_tensor(n, s, d))
        ps = lambda n, s, d: st.enter_context(nc.psum_tensor(n, s, d))
        W = sb("A_W", [128, 8, 2816], BF16)
        lng = sb("A_lng", [128, 512], F32)
        lnb = sb("A_lnb", [128, 512], F32)
        wraw = sb("A_wraw", [128, 4, 128], F32)
        wmT = sb("A_wmT", [128, 4, 128], BF16)
        tril = sb("A_tril", [128, 128], F32)
        biasB = sb("A_biasB", [128, 4, 4, 128], F32)
        xt = [sb("A_xt%d" % i, [128, D], F32) for i in range(2)]
        scr = sb("A_scr", [128, D], F32)
        ms = [sb("A_ms%d" % i, [128, 1], F32) for i in range(2)]
        hn = [sb("A_hn%d" % i, [128, D], BF16) for i in range(2)]
        hnT = [sb("A_hnT%d" % i, [128, 8, 512], BF16) for i in range(2)]
        vsb = sb("A_v", [128, 512], F32)
        vsq = sb("A_vsq", [128, 512], F32)
        mean4 = sb("A_mean4", [128, 4], F32)
        var4 = sb("A_var4", [128, 4], F32)
        vn = sb("A_vn", [128, 4, 512], BF16)
        st6 = sb("A_st6", [128, 4, 6], F32)
        mv = sb("A_mv", [128, 4, 2], F32)
        uT = [sb("A_uT%d" % i, [128, 512], F32) for i in range(2)]
        yaT = [sb("A_yaT%d" % i, [128, 512], F32) for i in range(2)]
        pbs = [sb("A_pbs%d" % i, [128, 512], F32) for i in range(2)]
        tp = [ps("A_tp%d" % i, [128, 8, 128], BF16) for i in range(2)]
        pv = ps("A_pv", [128, 512], F32)
        pu = [ps("A_pu%d" % i, [128, 512], F32) for i in range(2)]
        pm = ps("A_pm", [128, 4, 128], F32)
        pp = [ps("A_pp%d" % i, [128, 512], F32) for i in range(2)]

        win = I["e_w_in"].rearrange("(k p) n -> p k n", p=128)
        load_cast(k, W, win, 2816, w=["W"])
        k.dma(gt[:], I["e_norm_mix"][0, :].partition_broadcast(128), w=["gt"])
        k.dma(lng[:], I["sgu_ln_g"][0, :].partition_broadcast(128), w=["lng"])
        k.dma(lnb[:], I["sgu_ln_b"][0, :].partition_broadcast(128), w=["lnb"])
        k.dma(tril[:], I["c_tril"], w=["tril"])
        k.dma(wraw[:], I["sgu_w"].rearrange("g t s -> t g s"), w=["wraw"])
        for g in range(4):
            for j in range(4):
                k.dma(biasB[:, g, j, :], I["sgu_b"][g, :].partition_broadcast(128), w=["biasB"])
        for g in range(4):
            k.tt("dve", wraw[:, g, :], wraw[:, g, :], tril[:], ALU.mult, r=["wraw", "tril"], w=["wraw"])
        pw = pp[0]
        for g in range(4):
            k.tr(pw[:, g * 128:(g + 1) * 128], wraw[:, g, :], C["ident_f"][:], r=["wraw", "ident_f"], w=["pp0"])
        k.cp("dve", wmT[:].rearrange("p g t -> p (g t)"), pw[:], r=["pp0"], w=["wmT"])

        for grp in range(8):
            hp = grp % 2
            hT = hnT[hp]
            for ti in range(4):
                tile = grp * 4 + ti
                b = tile % 2
                tagb = "A%d" % b
                k.dma(xt[b][:], I["x"][tile * 128:(tile + 1) * 128, :], w=[tagb + "x"])
                rmsnorm_tile(k, xt[b][:], gt[:], hn[b][:], scr[:], ms[b][:], tagb)
                for kk in range(8):
                    k.tr(tp[b][:, kk, :], hn[b][:, kk * 128:(kk + 1) * 128], ident_bf[:], r=[tagb + "hn", "ident_bf"], w=["tp%d" % b])
                k.cp("act", hT[:, :, ti * 128:(ti + 1) * 128], tp[b][:], r=["tp%d" % b], w=["hnT%d" % hp])
            for ti in range(4):
                for kk in range(8):
                    k.mm(pv[:], hT[:, kk, ti * 128:(ti + 1) * 128], W[:, kk, 512:1024], kk == 0, kk == 7, r=["hnT%d" % hp, "W"], w=["pv"])
                k.act(vsb[:], pv[:], AF.Gelu_apprx_tanh, r=["pv"], w=["vsb"])
                V4 = vsb[:].rearrange("p (g d) -> p g d", d=128)
                P.add("dve", lambda e, V4=V4: e.tensor_reduce(out=mean4[:], in_=V4, op=ALU.add, axis=AX.X), reads=["vsb"], writes=["mean4"])
                k.ts("dve", mean4[:], mean4[:], 1.0 / 128, None, ALU.mult, r=["mean4"], w=["mean4"])
                k.tt("dve", V4, V4, mean4[:].unsqueeze(2).broadcast_to([128, 4, 128]), ALU.subtract, r=["vsb", "mean4"], w=["vsb"])
                k.tt("dve", vsq[:], vsb[:], vsb[:], ALU.mult, r=["vsb"], w=["vsq"])
                P.add("dve", lambda e: e.tensor_reduce(out=var4[:], in_=vsq[:].rearrange("p (g d) -> p g d", d=128), op=ALU.add, axis=AX.X), reads=["vsq"], writes=["var4"])
                k.ts("dve", var4[:], var4[:], 1.0 / 128, 1e-5, ALU.mult, ALU.add, r=["var4"], w=["var4"])
                k.act(var4[:], var4[:], AF.Sqrt, r=["var4"], w=["var4"])
                P.add("dve", lambda e: e.reciprocal(out=var4[:], in_=var4[:]), reads=["var4"], writes=["var4"])
                k.tt("dve", V4, V4, var4[:].unsqueeze(2).broadcast_to([128, 4, 128]), ALU.mult, r=["vsb", "var4"], w=["vsb"])
                k.tt("dve", vsb[:], vsb[:], lng[:], ALU.mult, r=["vsb", "lng"], w=["vsb"])
                k.tt("dve", vn[:, ti, :], vsb[:], lnb[:], ALU.add, r=["vsb", "lnb"], w=["vn"])
                if C.get("dbgout") is not None and grp == 0:
                    k.dma(C["dbgout"][ti * 128:(ti + 1) * 128, 0:512], vsb[:], r=["vsb"])
                    k.dma(C["dbgout"][ti * 128:(ti + 1) * 128, 512:516], mean4[:], r=["mean4"])
                    k.dma(C["dbgout"][ti * 128:(ti + 1) * 128, 516:520], var4[:], r=["var4"])
            for g in range(4):
                ub = g % 2
                for kk in range(8):
                    k.mm(pu[ub][:], W[:, kk, g * 128:(g + 1) * 128], hT[:, kk, :], kk == 0, kk == 7, r=["hnT%d" % hp, "W"], w=["pu%d" % ub])
                k.act(uT[ub][:], pu[ub][:], AF.Gelu_apprx_tanh, r=["pu%d" % ub], w=["uT%d" % ub])
                for ti in range(4):
                    k.mm(pm[:, ti, :], vn[:, ti, g * 128:(g + 1) * 128], wmT[:, g, :], True, True, r=["vn", "wmT"], w=["pm"])
                k.tt("dve", pbs[0][:], pm[:].rearrange("p a b -> p (a b)"), biasB[:, g, :, :].rearrange("p a b -> p (a b)"), ALU.add, r=["pm", "biasB"], w=["pbs0"])
                k.tt("dve", yaT[ub][:], pbs[0][:], uT[ub][:], ALU.mult, r=["pbs0", "uT%d" % ub], w=["yaT%d" % ub])
                k.dma(S["mix0T"][g, :, grp * 512:(grp + 1) * 512], yaT[ub][:], r=["yaT%d" % ub], w=[])
            for c in range(14):
                cb = c % 2
                for kk in range(8):
                    k.mm(pp[cb][:], W[:, kk, 1024 + c * 128:1024 + (c + 1) * 128], hT[:, kk, :], kk == 0, kk == 7, r=["hnT%d" % hp, "W"], w=["pp%d" % cb])
                k.cp("dve" if c % 2 else "act", pbs[cb][:], pp[cb][:], r=["pp%d" % cb], w=["pbs%d" % cb])
                k.dma(S["pbT"][c, :, grp * 512:(grp + 1) * 512], pbs[cb][:], r=["pbs%d" % cb])
        P.flush()


def stage_B(nc, k, P, I, S, C):
    with contextlib.ExitStack() as st:
        sb = lambda n, s, d: st.enter_context(nc.sbuf_tensor(n, s, d))
        ps = lambda n, s, d: st.enter_context(nc.psum_tensor(n, s, d))
        cols = sb("B_cols", [128, 30], F32)
        omk = sb("B_omk", [128, 4], F32)
        w2b = sb("B_w2b", [128, 1, 512], BF16)
        a2b = sb("B_a2b", [128, 1, 512], BF16)
        blk = sb("B_blk", [128, 128], F32)
        cur = [sb("B_cur%d" % i, [128, 516], F32) for i in range(2)]
        tmp = sb("B_tmp", [128, 512], F32)
        XS = sb("B_XS", [128, 14, 512], F32)
        TW = sb("B_TW", [128, 512], BF16)
        sgl = sb("B_sgl", [128, 512], F32)
        sg = sb("B_sg", [128, 512], F32)
        dec = sb("B_dec", [128, 512], F32)
        av = sb("B_av", [128, 512], F32)
        KK = sb("B_KK", [128, 512], F32)
        sq = sb("B_sq", [128, 512], F32)
        nrm = sb("B_nrm", [128, 512], F32)
        kap = sb("B_kap", [128, 512], F32)
        t1 = sb("B_t1", [128, 512], F32)
        kp = sb("B_kp", [128, 512], F32)
        nb = sb("B_nb", [128, 512], F32)
        tk = [sb("B_tk%d" % i, [128, 4, 128], F32) for i in range(2)]
        pw = ps("B_pw", [128, 512], F32)
        pa = ps("B_pa", [128, 512], F32)
        pss = ps("B_pss", [128, 512], F32)
        ptr = [ps("B_ptr%d" % i, [128, 4, 128], F32) for i in range(2)]
        identf = C["ident_f"]

        k.dma(cols[:], I["rw_cols"], w=["cols"])
        k.dma(w2b[0:64, 0, :], I["rwkv_w2"], w=["w2b"], eng="pool")
        k.dma(a2b[64:128, 0, :], I["rwkv_a2"], w=["a2b"], eng="pool")
        k.dma(blk[:], I["c_blk"], w=["blk"])
        k.ts("dve", omk[:], cols[:, 26:30], -1.0, 1.0, ALU.mult, ALU.add, r=["cols"], w=["omk"])
        MU, W0, A0, KKc, KA = 0, 14, 18, 22, 26
        ntr = 0
        for b in range(2):
            for tg in range(4):
                g0 = b * T + tg * 512
                for c in range(14):
                    cb = c % 2
                    if tg == 0:
                        k.memset("pool", cur[cb][:, 0:1], 0.0, w=["cur%d" % cb])
                        k.dma(cur[cb][:, 1:513], S["pbT"][c, :, g0:g0 + 512], w=["cur%d" % cb])
                    else:
                        k.dma(cur[cb][:, 0:513], S["pbT"][c, :, g0 - 1:g0 + 512], w=["cur%d" % cb])
                    k.tt("dve", tmp[:], cur[cb][:, 0:512], cur[cb][:, 1:513], ALU.subtract, r=["cur%d" % cb], w=["tmp"])
                    k.stt("dve", XS[:, c, :], tmp[:], cols[:, MU + c:MU + c + 1], cur[cb][:, 1:513], ALU.mult, ALU.add, r=["tmp", "cols", "cur%d" % cb], w=["XS%d" % c])
                k.act(TW[0:64, :], XS[0:64, 12, :], AF.Tanh, r=["XS12"], w=["TW"])
                k.cp("act", TW[64:128, :], XS[64:128, 12, :], r=["XS12"], w=["TW"])
                k.act(sgl[:], XS[:, 13, :], AF.Sigmoid, r=["XS13"], w=["sgl"])
                k.dma(S["sglT"][:, g0:g0 + 512], sgl[:], r=["sgl"])
                for c in range(4):
                    g = b * 4 + c
                    tsl = slice(tg * 512, tg * 512 + 512)
                    k.mm(pw[:], w2b[0:64, 0, c * 128:(c + 1) * 128], TW[0:64, :], True, True, r=["w2b", "TW"], w=["pw"])
                    k.act(sg[:], pw[:], AF.Sigmoid, r=["pw", "cols"], w=["sg"], bias=cols[:, W0 + c:W0 + c + 1])
                    k.act(dec[:], sg[:], AF.Exp, r=["sg"], w=["dec"], scale=-math.exp(-0.5))
                    k.dma(S["wT"][:, g, tsl], dec[:], r=["dec"])
                    k.mm(pa[:], a2b[64:128, 0, c * 128:(c + 1) * 128], TW[64:128, :], True, True, r=["a2b", "TW"], w=["pa"])
                    k.act(av[:], pa[:], AF.Sigmoid, r=["pa", "cols"], w=["av"], bias=cols[:, A0 + c:A0 + c + 1])
                    k.ts("dve", KK[:], XS[:, 4 + c, :], cols[:, KKc + c:KKc + c + 1], None, ALU.mult, r=["XS%d" % (4 + c), "cols"], w=["KK"])
                    k.tt("pool", sq[:], KK[:], KK[:], ALU.mult, r=["KK"], w=["sq"])
                    k.mm(pss[:], blk[:], sq[:], True, True, r=["blk", "sq"], w=["pss"])
                    k.act(nrm[:], pss[:], AF.Sqrt, r=["pss"], w=["nrm"])
                    k.ts("dve", nrm[:], nrm[:], 1e-12, None, ALU.max, r=["nrm"], w=["nrm"])
                    P.add("dve", lambda e: e.reciprocal(out=nrm[:], in_=nrm[:]), reads=["nrm"], writes=["nrm"])
                    k.tt("dve", kap[:], KK[:], nrm[:], ALU.mult, r=["KK", "nrm"], w=["kap"])
                    k.dma(S["kapT"][:, g, tsl], kap[:], r=["kap"])
                    k.dma(S["rsT"][:, g, tg * 512 + 1:tg * 512 + 513], XS[:, c, :], r=["XS%d" % c])
                    k.ts("dve", t1[:], av[:], cols[:, KA + c:KA + c + 1], omk[:, c:c + 1], ALU.mult, ALU.add, r=["av", "cols", "omk"], w=["t1"])
                    k.tt("pool", kp[:], XS[:, 4 + c, :], t1[:], ALU.mult, r=["XS%d" % (4 + c), "t1"], w=["kp"])
                    k.stt("dve", nb[:], kap[:], -1.0, av[:], ALU.mult, ALU.mult, r=["kap", "av"], w=["nb"])
                    for (src, skey, dst) in ((nb[:], "nb", "nb_tok"), (kp[:], "kp", "k_tok"), (XS[:, 8 + c, :], "XS%d" % (8 + c), "v_tok"), (XS[:, c, :], "XS%d" % c, "r_tok")):
                        pb_ = ntr % 2
                        ntr += 1
                        for ti in range(4):
                            k.tr(ptr[pb_][:, ti, :], src[:, ti * 128:(ti + 1) * 128], identf[:], r=[skey, "ident_f"], w=["ptr%d" % pb_])
                        k.cp("act" if pb_ else "dve", tk[pb_][:], ptr[pb_][:], r=["ptr%d" % pb_], w=["tk%d" % pb_])
                        k.dma(S[dst][tg * 512:tg * 512 + 512, g, :].rearrange("(ti p) c -> p ti c", p=128), tk[pb_][:], r=["tk%d" % pb_])
        P.flush()


def stage_C(nc, k, P, I, S, C):
    with contextlib.ExitStack() as st:
        sb = lambda n, s, d: st.enter_context(nc.sbuf_tensor(n, s, d))
        ps = lambda n, s, d: st.enter_context(nc.psum_tensor(n, s, d))
        ZU = [sb("C_ZU%d" % i, [128, 8, 4, 32], BF16) for i in range(2)]
        WW = [sb("C_WW%d" % i, [128, 8, 32], F32) for i in range(2)]
        LBK = [sb("C_LBK%d" % i, [128, 8, 128], BF16) for i in range(2)]
        VW = [sb("C_VW%d" % i, [128, 8, 64], F32) for i in range(2)]
        oh = sb("C_oh", [128, 32], F32)
        Wexp = [sb("C_Wexp%d" % i, [128, 32, 8, 64], F32) for i in range(2)]
        M = sb("C_M", [128, 8, 64], F32)
        Mb = sb("C_Mb", [128, 8, 64], BF16)
        Mw = [sb("C_Mw%d" % i, [128, 8, 64], F32) for i in range(2)]
        UV = [sb("C_UV%d" % i, [128, 8, 64], BF16) for i in range(2)]
        Ya = [sb("C_Ya%d" % i, [128, 8, 64], F32) for i in range(2)]
        rfin = sb("C_rfin", [128, 8, 2], F32)
        yfin = sb("C_yfin", [2, 8, 64], F32)
        pu = [ps("C_pu%d" % i, [128, 8, 64], F32) for i in range(2)]
        pm = [ps("C_pm%d" % i, [128, 8, 64], F32) for i in range(2)]
        R = lambda ap: ap

        k.dma(oh[:], I["c_oh"], w=["oh"])
        k.memset("pool", M[:], 0.0, w=["M"])
        k.memset("pool", Mb[:], 0.0, w=["Mb"])
        for i in range(2):
            k.memset("pool", ZU[i][:], 0.0, w=["ZU%d" % i])
            k.memset("pool", LBK[i][:], 0.0, w=["LB%d" % i, "LK%d" % i])
        k.memset("pool", rfin[:], 0.0, w=["rfin"])
        NW = T // 32
        for w_ in range(NW):
            wp = w_ % 2
            t0 = w_ * 32
            zk, wk, lbk, lkk, vk, yk = "ZU%d" % wp, "WW%d" % wp, "LB%d" % wp, "LK%d" % wp, "VW%d" % wp, "Ya%d" % wp
            k.dma(ZU[wp][0:64, :, 0, :], S["kapT"][0:64, :, t0:t0 + 32], w=[zk], eng="pool")
            k.dma(ZU[wp][64:128, :, 1, :], S["kapT"][64:128, :, t0:t0 + 32], w=[zk], eng="pool")
            if w_ == 0:
                k.memset("pool", ZU[wp][:, :, 2:4, 0:1], 0.0, w=[zk])
                k.dma(ZU[wp][0:64, :, 2, 1:32], S["rsT"][0:64, :, 1:32], w=[zk], eng="pool")
                k.dma(ZU[wp][64:128, :, 3, 1:32], S["rsT"][64:128, :, 1:32], w=[zk], eng="pool")
            else:
                k.dma(ZU[wp][0:64, :, 2, :], S["rsT"][0:64, :, t0:t0 + 32], w=[zk], eng="pool")
                k.dma(ZU[wp][64:128, :, 3, :], S["rsT"][64:128, :, t0:t0 + 32], w=[zk], eng="pool")
            k.dma(WW[wp][:], S["wT"][:, :, t0:t0 + 32], w=[wk])
            k.dma(LBK[wp][0:32, :, 0:64], S["nb_tok"][t0:t0 + 32, :, 0:64], w=[lbk], eng="pool")
            k.dma(LBK[wp][32:64, :, 64:128], S["nb_tok"][t0:t0 + 32, :, 64:128], w=[lbk], eng="pool")
            k.dma(LBK[wp][64:96, :, 0:64], S["k_tok"][t0:t0 + 32, :, 0:64], w=[lkk], eng="pool")
            k.dma(LBK[wp][96:128, :, 64:128], S["k_tok"][t0:t0 + 32, :, 64:128], w=[lkk], eng="pool")
            k.dma(VW[wp][64:96, :, :], S["v_tok"][t0:t0 + 32, :, 0:64], w=[vk])
            k.dma(VW[wp][96:128, :, :], S["v_tok"][t0:t0 + 32, :, 64:128], w=[vk])
            k.act(Wexp[wp][:], WW[wp][:].rearrange("p g m -> p m g").unsqueeze(3).broadcast_to([128, 32, 8, 64]), AF.Identity, r=[wk], w=["Wexp%d" % wp])
            k.memset("pool", Ya[wp][64:128, :, :], 0.0, w=[yk])
            for m in range(32):
                sp_ = m % 2
                k.tt("dve", Mw[sp_][:], M[:], Wexp[wp][:, m, :, :], ALU.mult, r=["M", "Wexp%d" % wp], w=["Mw%d" % sp_])
                k.act(UV[sp_][64:128, :, :], VW[wp][64:128, :, :], AF.Identity, r=[vk, "oh"], w=["Vs%d" % sp_], scale=oh[64:128, m:m + 1])
                for g in range(8):
                    k.mm(pu[sp_][:, g, :], R(ZU[wp][:, g, :, :].rearrange("p a b -> p (a b)")), Mb[:, g, :], True, True, r=[zk, "Mb"], w=["pu%d" % sp_])
                k.act(UV[sp_][0:64, :, :], pu[sp_][0:64, :, :], AF.Identity, r=["pu%d" % sp_, "oh"], w=["Us%d" % sp_], scale=oh[0:64, m:m + 1])
                k.stt("dve", Ya[wp][64:128, :, :], pu[sp_][64:128, :, :], oh[64:128, m:m + 1], Ya[wp][64:128, :, :], ALU.mult, ALU.add, r=["pu%d" % sp_, "oh", yk], w=[yk])
                for g in range(8):
                    k.mm(pm[sp_][:, g, :], LBK[wp][:, g, :], UV[sp_][:, g, :], True, True, r=[lbk, lkk, "Us%d" % sp_, "Vs%d" % sp_], w=["pm%d" % sp_])
                k.tt("dve", Mb[:], Mw[sp_][:], pm[sp_][:], ALU.add, r=["Mw%d" % sp_, "pm%d" % sp_], w=["Mb"])
                k.tt("dve", M[:], Mw[sp_][:], pm[sp_][:], ALU.add, r=["Mw%d" % sp_, "pm%d" % sp_], w=["M"])
            lo = 1 if w_ == 0 else 0
            k.dma(S["y_tok"][t0 - 1 + lo:t0 + 31, :, 0:64], Ya[wp][64 + lo:96, :, :], r=[yk])
            k.dma(S["y_tok"][t0 - 1 + lo:t0 + 31, :, 64:128], Ya[wp][96 + lo:128, :, :], r=[yk])
        k.dma(rfin[0:64, :, 0:1], S["rsT"][0:64, :, T:T + 1], w=["rfin"], slow=True)
        k.dma(rfin[64:128, :, 1:2], S["rsT"][64:128, :, T:T + 1], w=["rfin"], slow=True)
        for g in range(8):
            k.mm(pu[0][0:2, g, :], rfin[:, g, :], M[:, g, :], True, True, r=["rfin", "M"], w=["pu0"])
        k.cp("dve", yfin[:], pu[0][0:2, :, :], r=["pu0"], w=["yfin"])
        k.dma(S["y_tok"][T - 1:T, :, 0:64], yfin[0:1, :, :], r=["yfin"])
        k.dma(S["y_tok"][T - 1:T, :, 64:128], yfin[1:2, :, :], r=["yfin"])
        P.flush()


def stage_D(nc, k, P, I, S, C):
    ident_bf = C["ident_bf"]
    with contextlib.ExitStack() as st:
        sb = lambda n, s, d: st.enter_context(nc.sbuf_tensor(n, s, d))
        ps = lambda n, s, d: st.enter_context(nc.psum_tensor(n, s, d))
        Wo = sb("D_Wo", [128, 8, 1024], BF16)
        g2b = sb("D_g2b", [128, 1, 512], BF16)
        gng = sb("D_gng", [128, 512], F32)
        gnb = sb("D_gnb", [128, 512], F32)
        rkb = sb("D_rkb", [128, 512], F32)
        yt = [sb("D_yt%d" % i, [128, 512], F32) for i in range(2)]
        rt = [sb("D_rt%d" % i, [128, 512], F32) for i in range(2)]
        kt = [sb("D_kt%d" % i, [128, 512], F32) for i in range(2)]
        vt = [sb("D_vt%d" % i, [128, 512], F32) for i in range(2)]
        xt = [sb("D_xt%d" % i, [128, D], F32) for i in range(2)]
        sgt = [sb("D_sgt%d" % i, [128, 128], BF16) for i in range(2)]
        mixT = [sb("D_mixT%d" % i, [128, 8, 128], BF16) for i in range(2)]
        ysq = sb("D_ysq", [128, 512], F32)
        s1 = sb("D_s1", [128, 8], F32)
        s2 = sb("D_s2", [128, 8], F32)
        m2 = sb("D_m2", [128, 8], F32)
        s3 = sb("D_s3", [128, 8], F32)
        yb = sb("D_yb", [128, 512], BF16)
        h1 = sb("D_h1", [128, D], F32)
        pg = ps("D_pg", [128, 512], F32)
        pt = ps("D_pt", [128, 4, 128], BF16)
        po = [ps("D_po%d" % i, [128, 512], F32) for i in range(2)]

        load_cast(k, Wo, I["e_w_out"].rearrange("(k p) n -> p k n", p=128), 1024, w=["Wo"], step=1024)
        k.dma(g2b[:, 0, :], I["rwkv_g2"], w=["g2b"], eng="pool")
        k.dma(gng[:], I["rwkv_gn_g"][0, :].partition_broadcast(128), w=["gng"])
        k.dma(gnb[:], I["rwkv_gn_b"][0, :].partition_broadcast(128), w=["gnb"])
        k.dma(rkb[:], I["rwkv_r_k"][0, :].partition_broadcast(128), w=["rkb"])
        V3 = lambda ap: ap.rearrange("p (h i) -> p h i", i=64)
        B3 = lambda ap: ap.unsqueeze(2).broadcast_to([128, 8, 64]) if hasattr(ap, "unsqueeze") else ap
        for tile in range(32):
            b, tt, pb_ = tile // 16, tile % 16, tile % 2
            tk_ = "D%d" % pb_
            sl = slice(tt * 128, tt * 128 + 128)
            gs = slice(b * 4, b * 4 + 4)
            k.dma(yt[pb_][:].rearrange("p (g c) -> p g c", c=128), S["y_tok"][sl, gs, :], w=[tk_ + "y"])
            k.dma(rt[pb_][:].rearrange("p (g c) -> p g c", c=128), S["r_tok"][sl, gs, :], w=[tk_ + "r"])
            k.dma(kt[pb_][:].rearrange("p (g c) -> p g c", c=128), S["k_tok"][sl, gs, :], w=[tk_ + "k"])
            k.dma(vt[pb_][:].rearrange("p (g c) -> p g c", c=128), S["v_tok"][sl, gs, :], w=[tk_ + "v"])
            k.dma(xt[pb_][:], I["x"][tile * 128:(tile + 1) * 128, :], w=[tk_ + "x"])
            k.dma(sgt[pb_][:], S["sglT"][:, tile * 128:(tile + 1) * 128], w=[tk_ + "sg"], eng="pool")
            k.dma(mixT[pb_][:, 0:4, :], S["mix0T"][0:4, :, tile * 128:(tile + 1) * 128].rearrange("c p t -> p c t"), w=[tk_ + "mix"], eng="pool")
            Y, Rr, Kk, Vv = yt[pb_], rt[pb_], kt[pb_], vt[pb_]
            P.add("dve", lambda e, Y=Y: e.tensor_reduce(out=s1[:], in_=V3(Y[:]), op=ALU.add, axis=AX.X), reads=[tk_ + "y"], writes=["s1"])
            k.tt("pool", ysq[:], Y[:], Y[:], ALU.mult, r=[tk_ + "y"], w=["ysq"])
            P.add("dve", lambda e: e.tensor_reduce(out=s2[:], in_=V3(ysq[:]), op=ALU.add, axis=AX.X), reads=["ysq"], writes=["s2"])
            k.ts("dve", s1[:], s1[:], 1.0 / 64, None, ALU.mult, r=["s1"], w=["s1"])
            k.tt("dve", m2[:], s1[:], s1[:], ALU.mult, r=["s1"], w=["m2"])
            k.stt("dve", s2[:], s2[:], 1.0 / 64, m2[:], ALU.mult, ALU.subtract, r=["s2", "m2"], w=["s2"])
            k.ts("dve", s2[:], s2[:], 64e-5, None, ALU.add, r=["s2"], w=["s2"])
            k.act(s2[:], s2[:], AF.Sqrt, r=["s2"], w=["s2"])
            P.add("dve", lambda e: e.reciprocal(out=s2[:], in_=s2[:]), reads=["s2"], writes=["s2"])
            k.tt("dve", V3(Y[:]), V3(Y[:]), s1[:].unsqueeze(2).broadcast_to([128, 8, 64]), ALU.subtract, r=[tk_ + "y", "s1"], w=[tk_ + "y"])
            k.tt("dve", V3(Y[:]), V3(Y[:]), s2[:].unsqueeze(2).broadcast_to([128, 8, 64]), ALU.mult, r=[tk_ + "y", "s2"], w=[tk_ + "y"])
            k.tt("dve", Y[:], Y[:], gng[:], ALU.mult, r=[tk_ + "y", "gng"], w=[tk_ + "y"])
            k.tt("dve", Y[:], Y[:], gnb[:], ALU.add, r=[tk_ + "y", "gnb"], w=[tk_ + "y"])
            k.tt("pool", Rr[:], Rr[:], Kk[:], ALU.mult, r=[tk_ + "r", tk_ + "k"], w=[tk_ + "r"])
            k.tt("pool", Rr[:], Rr[:], rkb[:], ALU.mult, r=[tk_ + "r", "rkb"], w=[tk_ + "r"])
            P.add("dve", lambda e, Rr=Rr: e.tensor_reduce(out=s3[:], in_=V3(Rr[:]), op=ALU.add, axis=AX.X), reads=[tk_ + "r"], writes=["s3"])
            k.tt("dve", V3(Vv[:]), V3(Vv[:]), s3[:].unsqueeze(2).broadcast_to([128, 8, 64]), ALU.mult, r=[tk_ + "v", "s3"], w=[tk_ + "v"])
            k.tt("dve", Y[:], Y[:], Vv[:], ALU.add, r=[tk_ + "y", tk_ + "v"], w=[tk_ + "y"])
            k.mm(pg[:], sgt[pb_][:], g2b[:, 0, :], True, True, r=[tk_ + "sg", "g2b"], w=["pg"])
            k.tt("dve", yb[:], Y[:], pg[:], ALU.mult, r=[tk_ + "y", "pg"], w=["yb"])
            for c in range(4):
                k.tr(pt[:, c, :], yb[:, c * 128:(c + 1) * 128], ident_bf[:], r=["yb", "ident_bf"], w=["pt"])
            k.cp("act", mixT[pb_][:, 4:8, :], pt[:], r=["pt"], w=[tk_ + "mix"])
            for hf in range(2):
                for c in range(8):
                    k.mm(po[hf][:], mixT[pb_][:, c, :], Wo[:, c, hf * 512:(hf + 1) * 512], c == 0, c == 7, r=[tk_ + "mix", "Wo"], w=["po%d" % hf])
                k.tt("dve", h1[:, hf * 512:(hf + 1) * 512], xt[pb_][:, hf * 512:(hf + 1) * 512], po[hf][:], ALU.add, r=[tk_ + "x", "po%d" % hf], w=["h1"])
            k.dma(S["h"][tile * 128:(tile + 1) * 128, :], h1[:], r=["h1"])
        P.flush()


def ffn_dense(nc, k, P, S, C, pre, Wg_d, Wu_d, Wd_d, gain_d, nf, dst):
    ident_bf, gt = C["ident_bf"], C["gt"]
    F = nf * 128
    with contextlib.ExitStack() as st:
        sb = lambda n, s, d: st.enter_context(nc.sbuf_tensor(n, s, d))
        ps = lambda n, s, d: st.enter_context(nc.psum_tensor(n, s, d))
        Wg = sb(pre + "Wg", [128, 8, F], BF16)
        Wu = sb(pre + "Wu", [128, 8, F], BF16)
        Wd = sb(pre + "Wd", [128, nf, 1024], BF16)
        h1s = sb(pre + "h1s", [128, 4, D], F32)
        junk = sb(pre + "junk", [128, D], BF16)
        ms = sb(pre + "ms", [128, 1], F32)
        hn = sb(pre + "hn", [128, D], BF16)
        hnT = sb(pre + "hnT", [128, 8, 512], BF16)
        actT = sb(pre + "actT", [128, nf, 512], BF16)
        sil = sb(pre + "sil", [128, 512], F32)
        ho = sb(pre + "ho", [128, 512], F32)
        tp = ps(pre + "tp", [128, 8, 128], BF16)
        pg = [ps(pre + "pg%d" % i, [128, 512], F32) for i in range(2)]
        pu = [ps(pre + "pu%d" % i, [128, 512], F32) for i in range(2)]
        pd = [ps(pre + "pd%d" % i, [128, 512], F32) for i in range(2)]
        load_cast(k, Wg, Wg_d.rearrange("(k p) n -> p k n", p=128), F, w=["Wg"])
        load_cast(k, Wu, Wu_d.rearrange("(k p) n -> p k n", p=128), F, w=["Wu"])
        load_cast(k, Wd, Wd_d.rearrange("(f p) n -> p f n", p=128), 1024, w=["Wd"], step=1024)
        k.dma(gt[:], gain_d.partition_broadcast(128), w=["gt"])
        for grp in range(8):
            for ti in range(4):
                tile = grp * 4 + ti
                k.dma(h1s[:, ti, :], S["h"][tile * 128:(tile + 1) * 128, :], w=["h1s%d" % ti])
                k.act(junk[:], h1s[:, ti, :], AF.Square, r=["h1s%d" % ti], w=["junk", "ms"], accum_out=ms[:])
                k.ts("dve", ms[:], ms[:], 1.0 / D, 1e-6, ALU.mult, ALU.add, r=["ms"], w=["ms"])
                k.act(ms[:], ms[:], AF.Sqrt, r=["ms"], w=["ms"])
                P.add("dve", lambda e: e.reciprocal(out=ms[:], in_=ms[:]), reads=["ms"], writes=["ms"])
                k.stt("dve", hn[:], h1s[:, ti, :], ms[:], gt[:], ALU.mult, ALU.mult, r=["h1s%d" % ti, "ms", "gt"], w=["hn"])
                for kk in range(8):
                    k.tr(tp[:, kk, :], hn[:, kk * 128:(kk + 1) * 128], ident_bf[:], r=["hn", "ident_bf"], w=["tp"])
                k.cp("act", hnT[:, :, ti * 128:(ti + 1) * 128], tp[:], r=["tp"], w=["hnT"])
            for f in range(nf):
                fb = f % 2
                for kk in range(8):
                    k.mm(pg[fb][:], Wg[:, kk, f * 128:(f + 1) * 128], hnT[:, kk, :], kk == 0, kk == 7, r=["Wg", "hnT"], w=["pg%d" % fb])
                for kk in range(8):
                    k.mm(pu[fb][:], Wu[:, kk, f * 128:(f + 1) * 128], hnT[:, kk, :], kk == 0, kk == 7, r=["Wu", "hnT"], w=["pu%d" % fb])
                k.act(sil[:], pg[fb][:], AF.Silu, r=["pg%d" % fb], w=["sil"])
                k.tt("dve", actT[:, f, :], sil[:], pu[fb][:], ALU.mult, r=["sil", "pu%d" % fb], w=["actT"])
            for ti in range(4):
                tile = grp * 4 + ti
                for hf in range(2):
                    for f in range(nf):
                        k.mm(pd[hf][:], actT[:, f, ti * 128:(ti + 1) * 128], Wd[:, f, hf * 512:(hf + 1) * 512], f == 0, f == nf - 1, r=["actT", "Wd"], w=["pd%d" % hf])
                    k.tt("dve", ho[:], h1s[:, ti, hf * 512:(hf + 1) * 512], pd[hf][:], ALU.add, r=["h1s%d" % ti, "pd%d" % hf], w=["ho"])
                    k.dma(dst[tile * 128:(tile + 1) * 128, hf * 512:(hf + 1) * 512], ho[:], r=["ho"], w=["hdst%d" % tile])
        P.flush()


def stage_F1(nc, k, P, I, S, C):
    ident_bf, gt = C["ident_bf"], C["gt"]
    NCH = 30
    with contextlib.ExitStack() as st:
        sb = lambda n, s, d: st.enter_context(nc.sbuf_tensor(n, s, d))
        ps = lambda n, s, d: st.enter_context(nc.psum_tensor(n, s, d))
        W = sb("F_W", [128, 8, NCH * 128], BF16)
        Wt = sb("F_Wt", [128, 8, 280], BF16)
        ctab = sb("F_ctab", [128, T], F32)
        stab = sb("F_stab", [128, T], F32)
        ccol = sb("F_ccol", [128, 12], F32)
        xt = [sb("F_xt%d" % i, [128, D], F32) for i in range(2)]
        scr = sb("F_scr", [128, D], BF16)
        ms = [sb("F_ms%d" % i, [128, 1], F32) for i in range(2)]
        hn = [sb("F_hn%d" % i, [128, D], BF16) for i in range(2)]
        hnT = sb("F_hnT", [128, 8, 512], BF16)
        FM = sb("F_FM", [128, NCH, 512], F32)
        tmp = sb("F_tmp", [128, 512], F32)
        Z = [sb("F_Z%d" % i, [128, 514], F32) for i in range(4)]
        ycv = sb("F_ycv", [128, 512], F32)
        tko = [sb("F_tko%d" % i, [128, 280], F32) for i in range(2)]
        tp = [ps("F_tp%d" % i, [128, 8, 128], BF16) for i in range(2)]
        pf = [ps("F_pf%d" % i, [128, 512], F32) for i in range(2)]
        pk = [ps("F_pk%d" % i, [128, 280], F32) for i in range(2)]
        load_cast(k, W, I["o_w_fm"].rearrange("(k p) n -> p k n", p=128), NCH * 128, w=["W"], step=1280)
        load_cast(k, Wt, I["o_w_tm"].rearrange("(k p) n -> p k n", p=128), 280, w=["Wt"], step=280)
        k.dma(ctab[:], I["c_cos"], w=["ctab"])
        k.dma(stab[:], I["c_sin"], w=["stab"])
        k.dma(ccol[:], I["conv_cols"], w=["ccol"])
        k.dma(gt[:], I["o_norm_mix"][0, :].partition_broadcast(128), w=["gt"])
        for grp in range(8):
            bq, tg = grp // 4, grp % 4
            ts_ = slice(tg * 512, tg * 512 + 512)
            gs_ = slice(grp * 512, grp * 512 + 512)
            for ti in range(4):
                tile = grp * 4 + ti
                b = tile % 2
                tagb = "F%d" % b
                k.dma(xt[b][:], S["h"][tile * 128:(tile + 1) * 128, :], w=[tagb + "x"])
                rmsnorm_tile(k, xt[b][:], gt[:], hn[b][:], scr[:], ms[b][:], tagb)
                for kk in range(8):
                    k.tr(tp[b][:, kk, :], hn[b][:, kk * 128:(kk + 1) * 128], ident_bf[:], r=[tagb + "hn", "ident_bf"], w=["tp%d" % b])
                k.cp("act", hnT[:, :, ti * 128:(ti + 1) * 128], tp[b][:], r=["tp%d" % b], w=["hnT"])
            for c in range(NCH):
                cb = c % 2
                for kk in range(8):
                    k.mm(pf[cb][:], W[:, kk, c * 128:(c + 1) * 128], hnT[:, kk, :], kk == 0, kk == 7, r=["hnT", "W"], w=["pf%d" % cb])
                k.cp("act" if cb else "dve", FM[:, c, :], pf[cb][:], r=["pf%d" % cb], w=["FM%d" % c])
            for ti in range(4):
                tile = grp * 4 + ti
                tb = ti % 2
                for kk in range(8):
                    k.mm(pk[tb][:], hnT[:, kk, ti * 128:(ti + 1) * 128], Wt[:, kk, :], kk == 0, kk == 7, r=["hnT", "Wt"], w=["pk%d" % tb])
                k.cp("dve", tko[tb][:, 0:256], pk[tb][:, 0:256], r=["pk%d" % tb], w=["tko%d" % tb])
                k.act(tko[tb][:, 256:280], pk[tb][:, 256:280], AF.Sigmoid, r=["pk%d" % tb], w=["tko%d" % tb])
                k.dma(S["vs_tok"][tile * 128:(tile + 1) * 128, :], tko[tb][:, 0:128], r=["tko%d" % tb])
                k.dma(S["vw_tok"][tile * 128:(tile + 1) * 128, :], tko[tb][:, 128:256], r=["tko%d" % tb])
                k.dma(S["gates"][tile * 128:(tile + 1) * 128, :], tko[tb][:, 256:280], r=["tko%d" % tb])
            def rope(cx, cp_, dst):
                k.tt("pool", tmp[:], FM[:, cp_, :], stab[:, ts_], ALU.mult, r=["FM%d" % cp_, "stab"], w=["tmp"])
                k.tt("dve", FM[:, cp_, :], FM[:, cx, :], ctab[:, ts_], ALU.mult, r=["FM%d" % cx, "ctab"], w=["FM%d" % cp_])
                k.tt("dve", FM[:, cp_, :], FM[:, cp_, :], tmp[:], ALU.add, r=["FM%d" % cp_, "tmp"], w=["FM%d" % cp_])
                k.dma(dst, FM[:, cp_, :], r=["FM%d" % cp_])
            for c in range(4):
                k.dma(S["qT"][c, :, gs_], FM[:, c, :], r=["FM%d" % c])
                rope(c, 4 + c, S["qrT"][c, :, gs_])
            for c in range(2):
                rope(8 + c, 10 + c, S["ksr"][c, :, gs_])
                rope(12 + c, 14 + c, S["kwr"][c, :, gs_])
            k.dma(S["kcT"][:, gs_], FM[:, 16, :], r=["FM16"])
            k.dma(S["vcT"][:, gs_], FM[:, 17, :], r=["FM17"])
            for c in range(4):
                zk = "Z%d" % c
                if tg == 0:
                    k.memset("pool", Z[c][:, 0:2], 0.0, w=[zk])
                else:
                    k.cp("pool", Z[c][:, 0:2], Z[c][:, 512:514], r=[zk], w=[zk])
                k.tt("dve", Z[c][:, 2:514], FM[:, 22 + c, :], FM[:, 26 + c, :], ALU.mult, r=["FM%d" % (22 + c), "FM%d" % (26 + c), zk], w=[zk])
                k.ts("dve", ycv[:], Z[c][:, 0:512], ccol[:, c * 3:c * 3 + 1], None, ALU.mult, r=[zk, "ccol"], w=["ycv"])
                k.stt("dve", ycv[:], Z[c][:, 1:513], ccol[:, c * 3 + 1:c * 3 + 2], ycv[:], ALU.mult, ALU.add, r=[zk, "ccol", "ycv"], w=["ycv"])
                k.stt("dve", ycv[:], Z[c][:, 2:514], ccol[:, c * 3 + 2:c * 3 + 3], ycv[:], ALU.mult, ALU.add, r=[zk, "ccol", "ycv"], w=["ycv"])
                k.tt("dve", ycv[:], ycv[:], FM[:, 18 + c, :], ALU.mult, r=["ycv", "FM%d" % (18 + c)], w=["ycv"])
                k.dma(S["mix1T"][4 + c, :, gs_], ycv[:], r=["ycv"])
        P.flush()


def stage_F23(nc, k, P, I, S, C):
    ident_bf = C["ident_bf"]
    SC = 0.125
    with contextlib.ExitStack() as st:
        sb = lambda n, s, d: st.enter_context(nc.sbuf_tensor(n, s, d))
        ps = lambda n, s, d: st.enter_context(nc.psum_tensor(n, s, d))
        w1 = [sb("G_w1%d" % i, [128, 32, 256], BF16) for i in range(2)]
        pos = [sb("G_pos%d" % i, [128, 32], BF16) for i in range(2)]
        w2k = sb("G_w2k", [128, 2, 128], BF16)
        w2v = sb("G_w2v", [128, 2, 64], BF16)
        maskc = sb("G_maskc", [128, 16, 128], F32)
        fbias = sb("G_fbias", [128, 16, 32], F32)
        ovl = sb("G_ovl", [128, 1, 32], BF16)
        causal = sb("G_causal", [128, 128], F32)
        far = sb("G_far", [128, 128], F32)
        cvb = [sb("G_cvb%d" % i, [128, 2048], BF16) for i in range(2)]
        biasc = sb("G_biasc", [128, 4], F32)
        gh = sb("G_gh", [128, 2, 128], BF16)
        kcmp = [[sb("G_kcmp%d%d" % (b, h), [128, 128], BF16) for h in range(2)] for b in range(2)]
        vcmp = [[sb("G_vcmp%d%d" % (b, h), [128, 64], BF16) for h in range(2)] for b in range(2)]
        ksr = [sb("G_ksr%d" % i, [128, 2048], BF16) for i in range(2)]
        kwr = [sb("G_kwr%d" % i, [128, 2048], BF16) for i in range(2)]
        vsb = sb("G_vs", [128, 16, 128], BF16)
        vwb = sb("G_vw", [128, 16, 128], BF16)
        qT = [sb("G_qT%d" % i, [128, 4, 128], BF16) for i in range(2)]
        qrT = [sb("G_qrT%d" % i, [128, 4, 128], BF16) for i in range(2)]
        gat = [sb("G_gat%d" % i, [128, 24], F32) for i in range(2)]
        ssb = [sb("G_ssb%d" % i, [128, 2048], F32) for i in range(4)]
        pexp = [sb("G_pexp%d" % i, [128, 2048], BF16) for i in range(4)]
        pTs = [sb("G_pTs%d" % i, [128, 16, 128], BF16) for i in range(4)]
        pn = [sb("G_pn%d" % i, [128, 128], BF16) for i in range(4)]
        st_m = sb("G_m", [128, 4], F32)
        st_s = sb("G_s", [128, 4], F32)
        rs3 = sb("G_rs3", [128, 3, 4], F32)
        coef = sb("G_coef", [128, 8], F32)
        impb = sb("G_impb", [128, 32], F32)
        top8 = sb("G_top8", [128, 8], F32)
        selb = sb("G_selb", [128, 32], F32)
        osb = sb("G_osb", [128, 3, 256], F32)
        yc = sb("G_yc", [128, 512], F32)
        ycb = sb("G_ycb", [128, 512], BF16)
        ycT = sb("G_ycT", [128, 4, 128], F32)
        pS = [ps("G_pS%d" % i, [128, 512], F32) for i in range(2)]
        pT = [ps("G_pT%d" % i, [128, 8, 128], BF16) for i in range(2)]
        pO = ps("G_pO", [128, 3, 256], F32)
        pI = ps("G_pI", [128, 32], F32)

        for i, nm in enumerate(("nsa_k_w1", "nsa_v_w1")):
            src = I[nm].rearrange("(l d) c -> d l c", d=64)
            k.dma(w1[i][0:64, :, :], src, w=["w1%d" % i], eng="pool")
            k.dma(w1[i][64:128, :, :], src, w=["w1%d" % i], eng="pool")
        k.dma(pos[0][:], I["nsa_posk"], w=["pos0"], eng="pool")
        k.dma(pos[1][:], I["nsa_posv"], w=["pos1"], eng="pool")
        k.dma(w2k[:], I["nsa_k_w2d"].rearrange("(cc p) d -> p cc d", p=128), w=["w2k"], eng="pool")
        k.dma(w2v[:], I["nsa_v_w2"].rearrange("(cc p) d -> p cc d", p=128), w=["w2v"], eng="pool")
        k.dma(ovl[:, 0, :], I["c_ovl"], w=["ovl"], eng="pool")
        k.dma(maskc[:], I["c_maskc"], w=["maskc"])
        k.dma(fbias[:], I["c_fbias"], w=["fbias"])
        k.dma(causal[:], I["c_causal"], w=["causal"])
        k.dma(far[:], I["c_far"], w=["far"])
        k.memset("pool", gh[:], 0.0, w=["gh"])
        for b in range(2):
            for h in range(2):
                k.memset("pool", kcmp[b][h][:], 0.0, w=["kcmp%d%d" % (b, h)])
                k.memset("pool", vcmp[b][h][:], 0.0, w=["vcmp%d%d" % (b, h)])
        pB = pI
        for kv in range(2):
            for cc in range(2):
                j = kv * 2 + cc
                for l in range(32):
                    k.mm(pB[:, j:j + 1], w1[kv][0:64, l, cc * 128:(cc + 1) * 128], pos[kv][0:64, l:l + 1], l == 0, l == 31, r=["w1%d" % kv, "pos%d" % kv], w=["pI"])
        k.cp("dve", biasc[:], pB[:, 0:4], r=["pI"], w=["biasc"])
        for b in range(2):
            bs = slice(b * T, (b + 1) * T)
            k.dma(cvb[0][:], S["kcT"][:, bs], w=["cvb0"], eng="pool")
            k.dma(cvb[1][:], S["vcT"][:, bs], w=["cvb1"], eng="pool")
            for hk in range(2):
                base = hk * 64
                for kv in range(2):
                    for cc in range(2):
                        for l in range(32):
                            k.mm(pS[cc][:, 0:127], w1[kv][base:base + 64, l, cc * 128:(cc + 1) * 128], cvb[kv][base:base + 64, l:l + 2017:16], l == 0, l == 31, r=["w1%d" % kv, "cvb%d" % kv], w=["pS%d" % cc])
                        k.act(gh[:, cc, 0:127], pS[cc][:, 0:127], AF.Gelu_apprx_tanh, r=["pS%d" % cc, "biasc"], w=["gh"], bias=biasc[:, kv * 2 + cc:kv * 2 + cc + 1])
                    if kv == 0:
                        for cc in range(2):
                            k.mm(pO[:, 0, 0:127], w2k[:, cc, :], gh[:, cc, 0:127], cc == 0, cc == 1, r=["w2k", "gh"], w=["pO"])
                        k.cp("dve", kcmp[b][hk][:, 0:127], pO[:, 0, 0:127], r=["pO"], w=["kcmp%d%d" % (b, hk)])
                    else:
                        for cc in range(2):
                            k.mm(pO[0:127, 1, 0:64], gh[:, cc, 0:127], w2v[:, cc, :], cc == 0, cc == 1, r=["w2v", "gh"], w=["pO"])
                        k.cp("dve", vcmp[b][hk][0:127, :], pO[0:127, 1, 0:64], r=["pO"], w=["vcmp%d%d" % (b, hk)])

        def softmax4(items):
            for g, src, dst, so in items:
                P.add("dve", lambda e, g=g, src=src: e.tensor_reduce(out=st_m[:, g:g + 1], in_=src, op=ALU.max, axis=AX.X), reads=["ssrc%d" % g], writes=["st_m%d" % g])
            for g, src, dst, so in items:
                k.ts("dve", st_m[:, g:g + 1], st_m[:, g:g + 1], -30000.0, -1.0, ALU.max, ALU.mult, r=["st_m%d" % g], w=["st_m%d" % g])
            for g, src, dst, so in items:
                k.act(dst, src, AF.Exp, r=["ssrc%d" % g, "st_m%d" % g], w=["pexp%d" % g, "st_s%d" % g], bias=st_m[:, g:g + 1], accum_out=st_s[:, g:g + 1])
            for g, src, dst, so in items:
                k.ts("dve", st_s[:, g:g + 1], st_s[:, g:g + 1], 1e-30, None, ALU.max, r=["st_s%d" % g], w=["st_s%d" % g])
            for g, src, dst, so in items:
                P.add("dve", lambda e, g=g, so=so: e.reciprocal(out=so, in_=st_s[:, g:g + 1]), reads=["st_s%d" % g], writes=["rs3_%d" % g])

        def transposes(g, n_tiles):
            for t0 in range(0, n_tiles, 8):
                pb_ = (t0 // 8) % 2
                nt = min(8, n_tiles - t0)
                for j in range(nt):
                    k.tr(pT[pb_][:, j, :], pexp[g][:, (t0 + j) * 128:(t0 + j + 1) * 128], ident_bf[:], r=["pexp%d" % g, "ident_bf"], w=["pT%d" % pb_])
                k.cp("act" if pb_ else "dve", pTs[g][:, t0:t0 + nt, :], pT[pb_][:, 0:nt, :], r=["pT%d" % pb_], w=["pTs%d" % g])

        nsb = 0
        for b in range(2):
            bs = slice(b * T, (b + 1) * T)
            for hk in range(2):
                k.dma(ksr[hk][:], S["ksr"][hk, :, bs], w=["ksr%d" % hk], eng="pool")
                k.dma(kwr[hk][:], S["kwr"][hk, :, bs], w=["kwr%d" % hk], eng="pool")
            k.dma(vsb[:], S["vs_tok"][bs, :].rearrange("(t p) c -> p t c", p=128), w=["vsb"], eng="pool")
            k.dma(vwb[:], S["vw_tok"][bs, :].rearrange("(t p) c -> p t c", p=128), w=["vwb"], eng="pool")
            for qb in range(16):
                qp = qb % 2
                tsl = slice(b * T + qb * 128, b * T + qb * 128 + 128)
                k.dma(qT[qp][:], S["qT"][:, :, tsl].rearrange("c p t -> p c t"), w=["qT%d" % qp], eng="pool")
                k.dma(qrT[qp][:], S["qrT"][:, :, tsl].rearrange("c p t -> p c t"), w=["qrT%d" % qp], eng="pool")
                k.dma(gat[qp][:], S["gates"][tsl, :], w=["gat%d" % qp])
                nkt = qb + 1
                wlo = max(0, qb - 4)
                nwt = qb - wlo + 1
                for hk in range(2):
                    hd = lambda g: ((hk * 4 + g) // 2, ((hk * 4 + g) % 2) * 64)
                    for g in range(4):
                        ch, pbs_ = hd(g)
                        sp_ = nsb % 2
                        nsb += 1
                        k.mm(pS[sp_][:, 0:128], qT[qp][pbs_:pbs_ + 64, ch, :], kcmp[b][hk][pbs_:pbs_ + 64, :], True, True, r=["qT%d" % qp, "kcmp%d%d" % (b, hk)], w=["pS%d" % sp_])
                        k.stt("dve", ssb[g][:, 0:128], pS[sp_][:, 0:128], SC, maskc[:, qb, :], ALU.mult, ALU.add, r=["pS%d" % sp_, "maskc"], w=["ssrc%d" % g])
                    softmax4([(g, ssb[g][:, 0:128], pexp[g][:, 0:128], rs3[:, 0, g:g + 1]) for g in range(4)])
                    for g in range(4):
                        k.ts("dve", pn[g][:], pexp[g][:, 0:128], rs3[:, 0, g:g + 1], None, ALU.mult, r=["pexp%d" % g, "rs3_%d" % g], w=["pn%d" % g])
                    for g in range(4):
                        k.tr(pT[0][:, g, :], pn[g][:], ident_bf[:], r=["pn%d" % g, "ident_bf"], w=["pT0"])
                    k.cp("act", pTs[0][:, 0:4, :], pT[0][:, 0:4, :], r=["pT0"], w=["pTs0"])
                    for g in range(4):
                        k.mm(pO[:, 0, g * 64:(g + 1) * 64], pTs[0][:, g, :], vcmp[b][hk][:], True, True, r=["pTs0", "vcmp%d%d" % (b, hk)], w=["pO"])
                    for g in range(4):
                        k.mm(pI[:], pTs[0][:, g, :], ovl[:, 0, :], g == 0, g == 3, r=["pTs0", "ovl"], w=["pI"])
                    k.tt("dve", impb[:], pI[:], fbias[:, qb, :], ALU.add, r=["pI", "fbias"], w=["impb"])
                    P.add("dve", lambda e: e.max(out=top8[:], in_=impb[:]), reads=["impb"], writes=["top8"])
                    k.ts("dve", selb[:], impb[:], top8[:, 7:8], None, ALU.is_ge, r=["impb", "top8"], w=["selb"])
                    k.ts("dve", selb[:], selb[:], 1e30, -1e30, ALU.mult, ALU.add, r=["selb"], w=["selb"])
                    nk = nkt * 128
                    for g in range(4):
                        ch, pbs_ = hd(g)
                        for k0 in range(0, nk, 512):
                            k1 = min(nk, k0 + 512)
                            sp_ = nsb % 2
                            nsb += 1
                            k.mm(pS[sp_][:, 0:k1 - k0], qrT[qp][pbs_:pbs_ + 64, ch, :], ksr[hk][pbs_:pbs_ + 64, k0:k1], True, True, r=["qrT%d" % qp, "ksr%d" % hk], w=["pS%d" % sp_])
                            nb_ = (k1 - k0) // 64
                            k.stt("dve", ssb[g][:, k0:k1].rearrange("p (j c) -> p j c", c=64), pS[sp_][:, 0:k1 - k0].rearrange("p (j c) -> p j c", c=64), SC,
                                  selb[:, k0 // 64:k0 // 64 + nb_].unsqueeze(2).broadcast_to([128, nb_, 64]), ALU.mult, ALU.add, r=["pS%d" % sp_, "selb"], w=["ssrc%d" % g])
                        k.tt("pool", ssb[g][:, qb * 128:(qb + 1) * 128], ssb[g][:, qb * 128:(qb + 1) * 128], causal[:], ALU.add, r=["ssrc%d" % g, "causal"], w=["ssrc%d" % g])
                    softmax4([(g, ssb[g][:, 0:nk], pexp[g][:, 0:nk], rs3[:, 1, g:g + 1]) for g in range(4)])
                    for g in range(4):
                        transposes(g, nkt)
                        for kt in range(nkt):
                            k.mm(pO[:, 1, g * 64:(g + 1) * 64], pTs[g][:, kt, :], vsb[:, kt, hk * 64:(hk + 1) * 64], kt == 0, kt == nkt - 1, r=["pTs%d" % g, "vsb"], w=["pO"])
                    nk = nwt * 128
                    kbase = wlo * 128
                    for g in range(4):
                        ch, pbs_ = hd(g)
                        for k0 in range(0, nk, 512):
                            k1 = min(nk, k0 + 512)
                            sp_ = nsb % 2
                            nsb += 1
                            k.mm(pS[sp_][:, 0:k1 - k0], qrT[qp][pbs_:pbs_ + 64, ch, :], kwr[hk][pbs_:pbs_ + 64, kbase + k0:kbase + k1], True, True, r=["qrT%d" % qp, "kwr%d" % hk], w=["pS%d" % sp_])
                            k.act(ssb[g][:, k0:k1], pS[sp_][:, 0:k1 - k0], AF.Identity, r=["pS%d" % sp_], w=["ssrc%d" % g], scale=SC)
                        if qb >= 4:
                            k.tt("pool", ssb[g][:, 0:128], ssb[g][:, 0:128], far[:], ALU.add, r=["ssrc%d" % g, "far"], w=["ssrc%d" % g])
                        k.tt("pool", ssb[g][:, nk - 128:nk], ssb[g][:, nk - 128:nk], causal[:], ALU.add, r=["ssrc%d" % g, "causal"], w=["ssrc%d" % g])
                    softmax4([(g, ssb[g][:, 0:nk], pexp[g][:, 0:nk], rs3[:, 2, g:g + 1]) for g in range(4)])
                    for g in range(4):
                        transposes(g, nwt)
                        for kt in range(nwt):
                            k.mm(pO[:, 2, g * 64:(g + 1) * 64], pTs[g][:, kt, :], vwb[:, wlo + kt, hk * 64:(hk + 1) * 64], kt == 0, kt == nwt - 1, r=["pTs%d" % g, "vwb"], w=["pO"])
                    k.cp("act", osb[:], pO[:], r=["pO"], w=["osb"])
                    rk = ["rs3_%d" % g for g in range(4)]
                    for g in range(4):
                        h = hk * 4 + g
                        dst = yc[:, h * 64:(h + 1) * 64]
                        k.ts("dve", dst, osb[:, 0, g * 64:(g + 1) * 64], gat[qp][:, h * 3:h * 3 + 1], None, ALU.mult, r=["osb", "gat%d" % qp], w=["yc"])
                        for br in (1, 2):
                            k.tt("dve", coef[:, g * 2 + br - 1:g * 2 + br], gat[qp][:, h * 3 + br:h * 3 + br + 1], rs3[:, br, g:g + 1], ALU.mult, r=["gat%d" % qp] + rk, w=["coef"])
                            k.stt("dve", dst, osb[:, br, g * 64:(g + 1) * 64], coef[:, g * 2 + br - 1:g * 2 + br], dst, ALU.mult, ALU.add, r=["osb", "coef", "yc"], w=["yc"])
                k.cp("dve", ycb[:], yc[:], r=["yc"], w=["ycb"])
                for c in range(4):
                    k.tr(pT[1][:, c, :], ycb[:, c * 128:(c + 1) * 128], ident_bf[:], r=["ycb", "ident_bf"], w=["pT1"])
                k.cp("act", ycT[:], pT[1][:, 0:4, :], r=["pT1"], w=["ycT"])
                k.dma(S["mix1T"][0:4, :, tsl].rearrange("c p t -> p c t"), ycT[:], r=["ycT"])
        P.flush()


def stage_G(nc, k, P, I, S, C):
    with contextlib.ExitStack() as st:
        sb = lambda n, s, d: st.enter_context(nc.sbuf_tensor(n, s, d))
        ps = lambda n, s, d: st.enter_context(nc.psum_tensor(n, s, d))
        Wo = sb("O_Wo", [128, 8, 1024], BF16)
        xt = [sb("O_xt%d" % i, [128, D], F32) for i in range(2)]
        mixT = [sb("O_mixT%d" % i, [128, 8, 128], BF16) for i in range(2)]
        h1 = [sb("O_h1%d" % i, [128, D], F32) for i in range(2)]
        po = [ps("O_po%d" % i, [128, 512], F32) for i in range(2)]
        load_cast(k, Wo, I["o_w_out"].rearrange("(k p) n -> p k n", p=128), 1024, w=["Wo"], step=1024)
        for tile in range(32):
            pb_ = tile % 2
            tk_ = "O%d" % pb_
            sl = slice(tile * 128, (tile + 1) * 128)
            k.dma(xt[pb_][:], S["h"][sl, :], w=[tk_ + "x"])
            k.dma(mixT[pb_][:], S["mix1T"][:, :, sl].rearrange("c p t -> p c t"), w=[tk_ + "mix"], eng="pool")
            for hf in range(2):
                for c in range(8):
                    k.mm(po[hf][:], mixT[pb_][:, c, :], Wo[:, c, hf * 512:(hf + 1) * 512], c == 0, c == 7, r=[tk_ + "mix", "Wo"], w=["po%d" % hf])
                k.tt("dve", h1[pb_][:, hf * 512:(hf + 1) * 512], xt[pb_][:, hf * 512:(hf + 1) * 512], po[hf][:], ALU.add, r=[tk_ + "x", "po%d" % hf], w=[tk_ + "h1"])
            k.dma(S["h"][sl, :], h1[pb_][:], r=[tk_ + "h1"])
        P.flush()


def stage_H(nc, k, P, I, S, C, out):
    ident_f, gt = C["ident_f"], C["gt"]
    P.in_H = True
    NF = 11
    with contextlib.ExitStack() as st:
        sb = lambda n, s, d: st.enter_context(nc.sbuf_tensor(n, s, d))
        ps = lambda n, s, d: st.enter_context(nc.psum_tensor(n, s, d))
        Wg = sb("H_Wg", [128, 8, 1408], BF16)
        Wu = sb("H_Wu", [128, 8, 1408], BF16)
        Wd = sb("H_Wd", [128, NF, 1024], BF16)
        wr = sb("H_wr", [128, 8, 8], F32)
        brt = sb("H_brt", [128, 8], F32)
        gfin = sb("H_gfin", [128, D], F32)
        import os
        nq = int(os.environ.get("H_NQ", "16"))
        acc = sb("H_acc", [128, nq, D], F32)
        hnT = sb("H_hnT", [128, 8, nq * 128], BF16)
        wgt = sb("H_wgt", [128, 16, 8], F32)
        hnf = sb("H_hnf", [128, D], F32)
        hnTf = sb("H_hnTf", [128, 8, 128], F32)
        junk = sb("H_junk", [128, D], BF16)
        ms = sb("H_ms", [128, 1], F32)
        lg = sb("H_lg", [128, 8], F32)
        top8 = sb("H_top8", [128, 8], F32)
        g12 = sb("H_g12", [128, 2], F32)
        eq = sb("H_eq", [128, 8], F32)
        actT = sb("H_actT", [128, NF, 512], BF16)
        sil = sb("H_sil", [128, 512], F32)
        ho = [sb("H_ho%d" % i, [128, D], F32) for i in range(2)]
        ptf = ps("H_ptf", [128, 8, 128], F32)
        pr = ps("H_pr", [128, 8], F32)
        pg = ps("H_pg", [128, 512], F32)
        pu = ps("H_pu", [128, 512], F32)
        pd = [ps("H_pd%d" % i, [128, 512], F32) for i in range(2)]
        k.dma(wr[:], I["moe_router"].rearrange("(k p) e -> p k e", p=128), w=["wr"])
        k.dma(brt[:], I["moe_router_b"][0, :].partition_broadcast(128), w=["brt"])
        k.dma(gt[:], I["o_norm_ffn"][0, :].partition_broadcast(128), w=["gt"])
        k.dma(gfin[:], I["final_norm"][0, :].partition_broadcast(128), w=["gfin"])
        import os
        for half in range(int(os.environ.get("H_HALVES", "2"))):
            for ti in range(int(os.environ.get("H_TILES", "16"))):
                tile = half * 16 + ti
                ak = "acc%d" % ti
                k.dma(acc[:, ti, :], S["h"][tile * 128:(tile + 1) * 128, :], w=[ak])
                k.act(junk[:], acc[:, ti, :], AF.Square, r=[ak], w=["junk", "ms"], accum_out=ms[:])
                k.ts("dve", ms[:], ms[:], 1.0 / D, 1e-6, ALU.mult, ALU.add, r=["ms"], w=["ms"])
                k.act(ms[:], ms[:], AF.Sqrt, r=["ms"], w=["ms"])
                P.add("dve", lambda e: e.reciprocal(out=ms[:], in_=ms[:]), reads=["ms"], writes=["ms"])
                k.stt("dve", hnf[:], acc[:, ti, :], ms[:], gt[:], ALU.mult, ALU.mult, r=[ak, "ms", "gt"], w=["hnf"])
                for kk in range(8):
                    k.tr(ptf[:, kk, :], hnf[:, kk * 128:(kk + 1) * 128], ident_f[:], r=["hnf", "ident_f"], w=["ptf"])
                for bk in range(2):
                    ks_ = slice(bk * 4, bk * 4 + 4)
                    k.cp("act", hnTf[:, ks_, :], ptf[:, ks_, :], r=["ptf"], w=["hnTf"])
                    k.cp("dve", hnT[:, ks_, ti * 128:(ti + 1) * 128], hnTf[:, ks_, :], r=["hnTf"], w=["hnT"])
                if os.environ.get("H_SKIPR"):
                    continue
                for kk in range(8):
                    k.mm(pr[:], hnTf[:, kk, :], wr[:, kk, :], kk == 0, kk == 7, r=["hnTf", "wr"], w=["pr"])
                k.tt("dve", lg[:], pr[:], brt[:], ALU.add, r=["pr", "brt"], w=["lg"])
                P.add("dve", lambda e: e.max(out=top8[:], in_=lg[:]), reads=["lg"], writes=["top8"])
                k.tt("dve", g12[:, 0:1], top8[:, 0:1], top8[:, 1:2], ALU.subtract, r=["top8"], w=["g12"])
                k.tt("dve", g12[:, 1:2], top8[:, 1:2], top8[:, 0:1], ALU.subtract, r=["top8"], w=["g12"])
                k.act(g12[:], g12[:], AF.Sigmoid, r=["g12"], w=["g12"])
                k.ts("dve", eq[:], lg[:], top8[:, 0:1], g12[:, 0:1], ALU.is_equal, ALU.mult, r=["lg", "top8", "g12"], w=["eq"])
                k.ts("dve", wgt[:, ti, :], lg[:], top8[:, 1:2], g12[:, 1:2], ALU.is_equal, ALU.mult, r=["lg", "top8", "g12"], w=["wgt"])
                k.tt("dve", wgt[:, ti, :], wgt[:, ti, :], eq[:], ALU.add, r=["wgt", "eq"], w=["wgt"])
            import os
            hphase = int(os.environ.get("H_PHASE", "3"))
            for e in range(int(os.environ.get("H_NE", "8")) if hphase >= 2 else 0):
                k.dma(Wg[:], I["moe_w_gate"][e].rearrange("(k p) n -> p k n", p=128), w=["Wg"], eng="pool")
                k.dma(Wu[:], I["moe_w_up"][e].rearrange("(k p) n -> p k n", p=128), w=["Wu"], eng="pool")
                k.dma(Wd[:], I["moe_w_down"][e].rearrange("(f p) n -> p f n", p=128), w=["Wd"], eng="pool")
                for grp in range(4):
                    gsl = slice(grp * 512, (grp + 1) * 512)
                    for f in range(NF):
                        for kk in range(8):
                            k.mm(pg[:], Wg[:, kk, f * 128:(f + 1) * 128], hnT[:, kk, gsl], kk == 0, kk == 7, r=["Wg", "hnT"], w=["pg"])
                        for kk in range(8):
                            k.mm(pu[:], Wu[:, kk, f * 128:(f + 1) * 128], hnT[:, kk, gsl], kk == 0, kk == 7, r=["Wu", "hnT"], w=["pu"])
                        k.act(sil[:], pg[:], AF.Silu, r=["pg"], w=["sil"])
                        k.tt("dve", actT[:, f, :], sil[:], pu[:], ALU.mult, r=["sil", "pu"], w=["actT"])
                    for t4 in range(4):
                        ti = grp * 4 + t4
                        ak = "acc%d" % ti
                        for hf in range(2):
                            for f in range(NF):
                                k.mm(pd[hf][:], actT[:, f, t4 * 128:(t4 + 1) * 128], Wd[:, f, hf * 512:(hf + 1) * 512], f == 0, f == NF - 1, r=["actT", "Wd"], w=["pd%d" % hf])
                            k.stt("dve", acc[:, ti, hf * 512:(hf + 1) * 512], pd[hf][:], wgt[:, ti, e:e + 1], acc[:, ti, hf * 512:(hf + 1) * 512], ALU.mult, ALU.add, r=["pd%d" % hf, "wgt", ak], w=[ak])
            for ti in range(16 if int(os.environ.get("H_P3", "1")) else 0):
                tile = half * 16 + ti
                ak = "acc%d" % ti
                hb = ti % 2
                k.act(junk[:], acc[:, ti, :], AF.Square, r=[ak], w=["junk", "ms"], accum_out=ms[:])
                k.ts("dve", ms[:], ms[:], 1.0 / D, 1e-6, ALU.mult, ALU.add, r=["ms"], w=["ms"])
                k.act(ms[:], ms[:], AF.Sqrt, r=["ms"], w=["ms"])
                P.add("dve", lambda e: e.reciprocal(out=ms[:], in_=ms[:]), reads=["ms"], writes=["ms"])
                k.stt("dve", ho[hb][:], acc[:, ti, :], ms[:], gfin[:], ALU.mult, ALU.mult, r=[ak, "ms", "gfin"], w=["ho%d" % hb])
                k.dma(out[tile * 128:(tile + 1) * 128, :], ho[hb][:], r=["ho%d" % hb])
        P.flush()
```
